# Optimizing a Trainium2 kernel written in Bass

```python
import math
import jax
import jax.numpy as jnp
from jax import lax
import numpy as np

D_MODEL = 2048
BATCH = 4
SEQ = 4096
DEPTH = 4

CTX_LEN = 256
GRID_W = 64
N_MIXERS = 2

HEAD_DIM = 128
N_Q_HEADS = D_MODEL // HEAD_DIM
N_KV_HEADS = max(N_Q_HEADS // 4, 1)
Q_PER_KV = N_Q_HEADS // N_KV_HEADS
QKV_DIM = (N_Q_HEADS + 2 * N_KV_HEADS) * HEAD_DIM
WINDOW = 128
BLOCK = 128
AXIS_ROT = HEAD_DIM // 2
ROPE_BASE = 10000.0

SSM_GROUP = 16
SSM_GROUPS = D_MODEL // SSM_GROUP
SSM_STATE = 64
DT_MIN = 1e-3
DT_MAX = 1e-1

N_EXPERTS = 32
TOP_K = 4
D_EXPERT = D_MODEL // 2
SWIGLU_LIMIT = 7.0
SWIGLU_ALPHA = 1.702

EPS = 1e-6
NEG_INF = -1e30

kernel_name = 'hybrid_swa_s5_moe_diffusion_trunk'


def rms_norm(x, w):
    x32 = x.astype(jnp.float32)
    y = x32 * lax.rsqrt(jnp.mean(x32 * x32, axis=-1, keepdims=True) + EPS)
    return (y * w.astype(jnp.float32)).astype(x.dtype)


def head_rms(x, g):
    x32 = x.astype(jnp.float32)
    y = x32 * lax.rsqrt(jnp.mean(x32 * x32, axis=-1, keepdims=True) + EPS)
    return (y * g.astype(jnp.float32)).astype(x.dtype)


def modulate(h, shift, scale):
    return h * (1 + scale) + shift


def axial_rope_tables(n_rows):
    rows = jnp.repeat(jnp.arange(n_rows, dtype=jnp.float32), GRID_W)
    cols = jnp.tile(jnp.arange(GRID_W, dtype=jnp.float32), n_rows)
    inv_freq = ROPE_BASE ** (-jnp.arange(0, AXIS_ROT, 2, dtype=jnp.float32) / AXIS_ROT)
    ang = jnp.concatenate([rows[:, None] * inv_freq, cols[:, None] * inv_freq], axis=-1)
    return jnp.cos(ang), jnp.sin(ang)


def apply_axial_rope(x, cos, sin):
    b, l, h, d = x.shape
    half = AXIS_ROT // 2
    xr = x.astype(jnp.float32).reshape(b, l, h, 2, 2, half)
    x_a, x_b = xr[..., 0, :], xr[..., 1, :]
    cs = cos.reshape(l, 1, 2, half)
    sn = sin.reshape(l, 1, 2, half)
    out = jnp.stack([x_a * cs - x_b * sn, x_b * cs + x_a * sn], axis=-2)
    return out.reshape(b, l, h, d).astype(x.dtype)


def windowed_gqa_attention(u_lat, u_ctx, w_qkv, w_o, q_gain, k_gain, sinks, cos, sin, ctx_out):
    b, l, d = u_lat.shape
    lc = u_ctx.shape[1]
    nb = l // BLOCK
    scale = HEAD_DIM ** -0.5
    nq = N_Q_HEADS * HEAD_DIM
    nk = N_KV_HEADS * HEAD_DIM

    def project(u):
        n = u.shape[1]
        qkv = u @ w_qkv
        q = qkv[..., :nq].reshape(b, n, N_Q_HEADS, HEAD_DIM)
        k = qkv[..., nq:nq + nk].reshape(b, n, N_KV_HEADS, HEAD_DIM)
        v = qkv[..., nq + nk:].reshape(b, n, N_KV_HEADS, HEAD_DIM)
        return head_rms(q, q_gain), head_rms(k, k_gain), v

    q_l, k_l, v_l = project(u_lat)
    q_c, k_c, v_c = project(u_ctx)
    q_l = apply_axial_rope(q_l, cos, sin)
    k_l = apply_axial_rope(k_l, cos, sin)
    sink_logit = sinks.astype(jnp.float32).reshape(N_KV_HEADS, Q_PER_KV)

    qb = q_l.reshape(b, nb, BLOCK, N_KV_HEADS, Q_PER_KV, HEAD_DIM)
    pad = ((0, 0), (BLOCK, BLOCK), (0, 0), (0, 0))
    kp = jnp.pad(k_l, pad).reshape(b, nb + 2, BLOCK, N_KV_HEADS, HEAD_DIM)
    vp = jnp.pad(v_l, pad).reshape(b, nb + 2, BLOCK, N_KV_HEADS, HEAD_DIM)
    k_band = jnp.concatenate([kp[:, :-2], kp[:, 1:-1], kp[:, 2:]], axis=2)
    v_band = jnp.concatenate([vp[:, :-2], vp[:, 1:-1], vp[:, 2:]], axis=2)
    s_loc = jnp.einsum('bnqkgd,bnskd->bnkgqs', qb, k_band).astype(jnp.float32) * scale
    qpos = jnp.arange(nb)[:, None, None] * BLOCK + jnp.arange(BLOCK)[None, :, None]
    kpos = (jnp.arange(nb)[:, None, None] - 1) * BLOCK + jnp.arange(3 * BLOCK)[None, None, :]
    valid = (jnp.abs(kpos - qpos) <= WINDOW) & (kpos >= 0) & (kpos < l)
    s_loc = jnp.where(valid[None, :, None, None], s_loc, NEG_INF)
    s_ctx = jnp.einsum('bnqkgd,bskd->bnkgqs', qb, k_c).astype(jnp.float32) * scale
    sink_col = jnp.broadcast_to(sink_logit[None, None, :, :, None, None], s_loc.shape[:-1] + (1,))
    p = jax.nn.softmax(jnp.concatenate([s_loc, s_ctx, sink_col], axis=-1), axis=-1).astype(v_l.dtype)
    o = (jnp.einsum('bnkgqs,bnskd->bnqkgd', p[..., :3 * BLOCK], v_band)
         + jnp.einsum('bnkgqs,bskd->bnqkgd', p[..., 3 * BLOCK:3 * BLOCK + lc], v_c))
    y_lat = o.reshape(b, l, nq) @ w_o

    y_ctx = None
    if ctx_out:
        qc = q_c.reshape(b, lc, N_KV_HEADS, Q_PER_KV, HEAD_DIM)
        s_cc = jnp.einsum('bqkgd,bskd->bkgqs', qc, k_c).astype(jnp.float32) * scale
        sink_cc = jnp.broadcast_to(sink_logit[None, :, :, None, None], s_cc.shape[:-1] + (1,))
        pc = jax.nn.softmax(jnp.concatenate([s_cc, sink_cc], axis=-1), axis=-1).astype(v_c.dtype)
        oc = jnp.einsum('bkgqs,bskd->bqkgd', pc[..., :lc], v_c)
        y_ctx = oc.reshape(b, lc, nq) @ w_o
    return y_lat, y_ctx


def ssm_scan(bu, lam_dt, reverse, h0=None):
    n = bu.shape[1]
    steps = jnp.ones((1, n, 1, 1), jnp.float32)

    def combine(left, right):
        k1, h1 = left
        k2, h2 = right
        return k1 + k2, jnp.exp(lam_dt * k2) * h1 + h2

    _, h = lax.associative_scan(combine, (steps, bu), reverse=reverse, axis=1)
    if h0 is not None:
        t = jnp.arange(n, dtype=jnp.float32)
        count = (n - t) if reverse else (t + 1.0)
        h = h + jnp.exp(lam_dt * count[:, None, None]) * h0[:, None]
    return h


def s5_mixer(u_lat, u_ctx, a_re, a_im, log_dt, b_re, b_im, c_re, c_im, d_skip, w_glu, b_glu, ctx_out):
    b, l, d = u_lat.shape
    lc = u_ctx.shape[1]
    lam = lax.complex(jnp.minimum(a_re.astype(jnp.float32), -1e-4), a_im.astype(jnp.float32))
    lam_dt = lam * jnp.exp(log_dt.astype(jnp.float32))[..., None]
    b_bar = ((jnp.exp(lam_dt) - 1.0) / lam)[..., None] * lax.complex(
        b_re.astype(jnp.float32), b_im.astype(jnp.float32))
    c_mat = lax.complex(c_re.astype(jnp.float32), c_im.astype(jnp.float32))
    ug_l = u_lat.astype(jnp.float32).reshape(b, l, SSM_GROUPS, SSM_GROUP).astype(jnp.complex64)
    ug_c = u_ctx.astype(jnp.float32).reshape(b, lc, SSM_GROUPS, SSM_GROUP).astype(jnp.complex64)
    dsk = d_skip.astype(jnp.float32)
    y_l = dsk * u_lat.astype(jnp.float32)
    y_c = dsk * u_ctx.astype(jnp.float32)
    for direction, reverse in ((0, False), (1, True)):
        bu_c = jnp.einsum('bsgp,gnp->bsgn', ug_c, b_bar[direction])
        h_c = ssm_scan(bu_c, lam_dt[direction], reverse)
        h0 = h_c[:, 0] if reverse else h_c[:, -1]
        bu_l = jnp.einsum('bsgp,gnp->bsgn', ug_l, b_bar[direction])
        h_l = ssm_scan(bu_l, lam_dt[direction], reverse, h0)
        y_l = y_l + jnp.einsum('bsgn,gpn->bsgp', h_l, c_mat[direction]).real.reshape(b, l, d)
        if ctx_out:
            y_c = y_c + jnp.einsum('bsgn,gpn->bsgp', h_c, c_mat[direction]).real.reshape(b, lc, d)

    def glu(y):
        g = jax.nn.gelu(y).astype(u_lat.dtype)
        z = g @ w_glu + b_glu
        return z[..., :d] * jax.nn.sigmoid(z[..., d:])

    return glu(y_l), (glu(y_c) if ctx_out else None)


def moe_ffn(x2d, w_router, b_router, w_gu, b_gu, w_down, b_down):
    logits = x2d.astype(jnp.float32) @ w_router.astype(jnp.float32) + b_router.astype(jnp.float32)
    top_val, top_idx = lax.top_k(logits, TOP_K)
    top_w = jax.nn.softmax(top_val, axis=-1)
    gates = jnp.sum(jax.nn.one_hot(top_idx, N_EXPERTS, dtype=jnp.float32) * top_w[..., None], axis=1)

    def expert_step(acc, params):
        wgu, bgu, wdn, bdn, g = params
        h = x2d @ wgu + bgu
        gate = jnp.minimum(h[..., 0::2], SWIGLU_LIMIT)
        up = jnp.clip(h[..., 1::2], -SWIGLU_LIMIT, SWIGLU_LIMIT)
        act = (up + 1) * (gate * jax.nn.sigmoid(SWIGLU_ALPHA * gate))
        y = act @ wdn + bdn
        return acc + g[:, None] * y.astype(jnp.float32), None

    acc0 = jnp.zeros(x2d.shape, jnp.float32)
    acc, _ = lax.scan(expert_step, acc0, (w_gu, b_gu, w_down, b_down, gates.T))
    return acc.astype(x2d.dtype)


def setup_inputs(seed: int = 0) -> dict:
    key = jax.random.key(seed)
    ks = jax.random.split(key, 32)
    f32 = jnp.float32
    n_attn = (DEPTH + 1) // 2
    n_ssm = DEPTH // 2
    D = D_MODEL

    def nrm(k, shape, s):
        return s * jax.random.normal(k, shape, f32)

    a_im_base = jnp.pi * jnp.arange(SSM_STATE, dtype=f32)
    return {
        'x': nrm(ks[0], (BATCH, SEQ, D), 1.0),
        'c': nrm(ks[1], (BATCH, D), 1.0),
        'ctx': nrm(ks[2], (BATCH, CTX_LEN, D), 1.0),
        'c_ctx': nrm(ks[3], (D,), 1.0),
        'w_mod': nrm(ks[4], (DEPTH, D, 6 * D), 0.5 * D ** -0.5),
        'b_mod': nrm(ks[5], (DEPTH, 6 * D), 0.02),
        'norm_mix': 1.0 + nrm(ks[6], (DEPTH, D), 0.05),
        'norm_ffn': 1.0 + nrm(ks[7], (DEPTH, D), 0.05),
        'w_router': nrm(ks[8], (DEPTH, D, N_EXPERTS), D ** -0.5),
        'b_router': nrm(ks[9], (DEPTH, N_EXPERTS), 0.01),
        'w_gate_up': nrm(ks[10], (DEPTH, N_EXPERTS, D, 2 * D_EXPERT), D ** -0.5),
        'b_gate_up': nrm(ks[11], (DEPTH, N_EXPERTS, 2 * D_EXPERT), 0.02),
        'w_down': nrm(ks[12], (DEPTH, N_EXPERTS, D_EXPERT, D), D_EXPERT ** -0.5),
        'b_down': nrm(ks[13], (DEPTH, N_EXPERTS, D), 0.02),
        'attn_w_qkv': nrm(ks[14], (n_attn, D, QKV_DIM), D ** -0.5),
        'attn_w_o': nrm(ks[15], (n_attn, N_Q_HEADS * HEAD_DIM, D), (N_Q_HEADS * HEAD_DIM) ** -0.5),
        'attn_q_gain': 1.0 + nrm(ks[16], (n_attn, HEAD_DIM), 0.05),
        'attn_k_gain': 1.0 + nrm(ks[17], (n_attn, HEAD_DIM), 0.05),
        'attn_sinks': nrm(ks[18], (n_attn, N_Q_HEADS), 0.5),
        'ssm_a_re': -0.5 + nrm(ks[19], (n_ssm, 2, SSM_GROUPS, SSM_STATE), 0.01),
        'ssm_a_im': a_im_base + nrm(ks[20], (n_ssm, 2, SSM_GROUPS, SSM_STATE), 0.01),
        'ssm_log_dt': jax.random.uniform(ks[21], (n_ssm, 2, SSM_GROUPS), f32,
                                         math.log(DT_MIN), math.log(DT_MAX)),
        'ssm_b_re': nrm(ks[22], (n_ssm, 2, SSM_GROUPS, SSM_STATE, SSM_GROUP), (2 * SSM_GROUP) ** -0.5),
        'ssm_b_im': nrm(ks[23], (n_ssm, 2, SSM_GROUPS, SSM_STATE, SSM_GROUP), (2 * SSM_GROUP) ** -0.5),
        'ssm_c_re': nrm(ks[24], (n_ssm, 2, SSM_GROUPS, SSM_GROUP, SSM_STATE), SSM_STATE ** -0.5),
        'ssm_c_im': nrm(ks[25], (n_ssm, 2, SSM_GROUPS, SSM_GROUP, SSM_STATE), SSM_STATE ** -0.5),
        'ssm_d': nrm(ks[26], (n_ssm, D), 1.0),
        'ssm_w_glu': nrm(ks[27], (n_ssm, D, 2 * D), D ** -0.5),
        'ssm_b_glu': nrm(ks[28], (n_ssm, 2 * D), 0.02),
    }


def reference(x, c, ctx, c_ctx, w_mod, b_mod, norm_mix, norm_ffn, w_router, b_router,
              w_gate_up, b_gate_up, w_down, b_down, attn_w_qkv, attn_w_o, attn_q_gain,
              attn_k_gain, attn_sinks, ssm_a_re, ssm_a_im, ssm_log_dt, ssm_b_re, ssm_b_im,
              ssm_c_re, ssm_c_im, ssm_d, ssm_w_glu, ssm_b_glu):
    b, seq, d = x.shape
    lc = ctx.shape[1]
    n_rows = seq // GRID_W
    cos, sin = axial_rope_tables(n_rows)
    silu_c = jax.nn.silu(c)
    silu_cc = jax.nn.silu(c_ctx)
    for i in range(DEPTH):
        last = i == DEPTH - 1
        j = i // N_MIXERS
        mod_l = silu_c @ w_mod[i] + b_mod[i]
        mod_c = silu_cc @ w_mod[i] + b_mod[i]
        sh_m, sc_m, g_m, sh_f, sc_f, g_f = jnp.split(mod_l[:, None, :], 6, axis=-1)
        csh_m, csc_m, cg_m, csh_f, csc_f, cg_f = jnp.split(mod_c, 6)
        u_l = modulate(rms_norm(x, norm_mix[i]), sh_m, sc_m)
        u_c = modulate(rms_norm(ctx, norm_mix[i]), csh_m, csc_m)
        if i % N_MIXERS == 0:
            o_l, o_c = windowed_gqa_attention(u_l, u_c, attn_w_qkv[j], attn_w_o[j], attn_q_gain[j],
                                              attn_k_gain[j], attn_sinks[j], cos, sin, not last)
        else:
            o_l, o_c = s5_mixer(u_l, u_c, ssm_a_re[j], ssm_a_im[j], ssm_log_dt[j], ssm_b_re[j],
                                ssm_b_im[j], ssm_c_re[j], ssm_c_im[j], ssm_d[j], ssm_w_glu[j],
                                ssm_b_glu[j], not last)
        x = x + g_m * o_l
        h_l = modulate(rms_norm(x, norm_ffn[i]), sh_f, sc_f).reshape(b * seq, d)
        moe_params = (w_router[i], b_router[i], w_gate_up[i], b_gate_up[i], w_down[i], b_down[i])
        if last:
            f_l = moe_ffn(h_l, *moe_params)
        else:
            ctx = ctx + cg_m * o_c
            h_c = modulate(rms_norm(ctx, norm_ffn[i]), csh_f, csc_f).reshape(b * lc, d)
            f = moe_ffn(jnp.concatenate([h_l, h_c], axis=0), *moe_params)
            f_l = f[:b * seq]
            ctx = ctx + cg_f * f[b * seq:].reshape(b, lc, d)
        x = x + g_f * f_l.reshape(b, seq, d)
    return x
```

```python
import numpy as np
import concourse.bass as bass
import concourse.mybir as mybir
from concourse.bass_utils import run_bass_kernel_spmd

F32, BF16 = mybir.dt.float32, mybir.dt.bfloat16
AF = mybir.ActivationFunctionType
ALU = mybir.AluOpType

D = 2048
NE = 32
FE = 1024
EPS = 1e-6


class Prog:
    NDS = 24

    def __init__(self):
        self.nc = bass.Bass("TRN2", target_bir_lowering=False)
        nc = self.nc
        self.E = {"pe": nc.tensor, "act": nc.scalar, "dve": nc.vector, "pool": nc.gpsimd, "sp": nc.sync}
        self.sem = {e: nc.semaphore("sem_" + e).__enter__() for e in ("pe", "act", "dve", "pool")}
        self.cnt = {e: 0 for e in self.sem}
        self.waited = {}
        self.lastw = {}
        self.readers = {}
        self.dsems = [nc.semaphore("dsem%d" % i).__enter__() for i in range(self.NDS)]
        self.dcount = [0] * self.NDS
        self.dnext = 0
        self.nbank = 0

    def sb(self, name, shape, dt=F32):
        return self.nc.sbuf_tensor("s_" + name, list(shape), dt).__enter__()

    def bank(self, name=None):
        self.nbank += 1
        return self.nc.psum_tensor(name or ("bank%d" % self.nbank), [128, 512], F32).__enter__()

    def dram_in(self, name, shape, dt=F32):
        return self.nc.dram_tensor(name, list(shape), dt, kind="ExternalInput").ap()

    def dram_out(self, name, shape, dt=F32):
        return self.nc.dram_tensor(name, list(shape), dt, kind="ExternalOutput").ap()

    def _deps(self, reads, writes):
        toks = []
        for r in reads:
            if r in self.lastw:
                toks.append(self.lastw[r])
        for w in writes:
            if w in self.lastw:
                toks.append(self.lastw[w])
            toks.extend(self.readers.get(w, {}).values())
        return toks

    def _wait(self, e, toks):
        need = {}
        for s, v in toks:
            k = id(s)
            if k not in need or need[k][1] < v:
                need[k] = (s, v)
        for k, (s, v) in need.items():
            if self.waited.get((e, k), 0) < v:
                self.E[e].wait_ge(s, v)
                self.waited[(e, k)] = v

    def _commit(self, tok, reads, writes):
        for w in writes:
            self.lastw[w] = tok
            self.readers[w] = {}
        for r in reads:
            d = self.readers.setdefault(r, {})
            k = id(tok[0])
            if k not in d or d[k][1] < tok[1]:
                d[k] = tok

    def op(self, e, fn, reads=(), writes=()):
        toks = self._deps(reads, writes)
        if e == "pe":
            toks = [t for t in toks if t[0] is not self.sem["pe"]]
        self._wait(e, toks)
        inst = fn(self.E[e])
        self.cnt[e] += 1
        inst.then_inc(self.sem[e], 1)
        self._commit((self.sem[e], self.cnt[e]), reads, writes)

    def dma(self, q, out, in_, reads=(), writes=(), **kw):
        toks = self._deps(reads, writes)
        slot = self.dnext
        self.dnext = (slot + 1) % self.NDS
        if self.dcount[slot] > 0:
            toks.append((self.dsems[slot], 16 * self.dcount[slot]))
        self._wait(q, toks)
        self.E[q].dma_start(out=out, in_=in_, **kw).then_inc(self.dsems[slot], 16)
        self.dcount[slot] += 1
        self._commit((self.dsems[slot], 16 * self.dcount[slot]), reads, writes)

    def finish(self):
        toks = [(self.dsems[i], 16 * self.dcount[i]) for i in range(self.NDS) if self.dcount[i] > 0]
        self._wait("sp", toks)
        return self.nc


def build_moe(NT=17, n_lat=16, ne=NE, passes=None):
    P = Prog()
    nc = P.nc
    if passes is None:
        passes = [list(range(0, 5)), list(range(5, 9)), list(range(9, 13)), list(range(13, 17))]
    x_d = P.dram_in("x", [NT * 128, D])
    nw_d = P.dram_in("nw_b", [128, D])
    sc_d = P.dram_in("sc_b", [2, 128, D])
    sh_d = P.dram_in("sh_b", [2, 128, D])
    gf_d = P.dram_in("gf_b", [2, 128, D])
    wr_d = P.dram_in("wr", [128, 16, ne])
    br_d = P.dram_in("br_b", [128, ne])
    wgu_d = P.dram_in("wgu", [ne, D, 2 * FE])
    bgu_d = P.dram_in("bgu", [128, ne, 2, 8])
    wdn_d = P.dram_in("wdn", [ne, FE, D])
    bdn_d = P.dram_in("bdn", [ne, D])
    id_d = P.dram_in("ident", [128, 128])
    y_d = P.dram_out("y", [NT * 128, D])

    maxt = max(len(p) for p in passes)
    ident = P.sb("ident", [128, 128])
    nw_b = P.sb("nw_b_s", [128, D])
    A_b = P.sb("A_b", [128, D])
    B_b = P.sb("B_b", [128, D])
    G_b = [P.sb("G_b%d" % s, [128, D]) for s in range(2)]
    wr = P.sb("wr_s", [128, 16, ne])
    br_b = P.sb("br_s", [128, ne])
    bgu = P.sb("bgu_s", [128, ne, 2, 8])
    bdn = P.sb("bdn_s", [ne, D])
    hT = P.sb("hT", [128, 16, maxt * 128], BF16)
    actT = P.sb("actT", [128, 8, maxt * 128], BF16)
    acc = P.sb("acc", [128, maxt, D])
    gates = P.sb("gates", [128, maxt, ne])
    gT = P.sb("gT", [ne, maxt * 128])
    xt = P.sb("xt", [128, D])
    h32T = P.sb("h32T", [128, 16, 128])
    lg = P.sb("lg", [128, ne])
    m8 = P.sb("m8", [128, 8])
    sm = P.sb("sm", [128, 8])
    NGU, NDN = 3, 3
    wg = [P.sb("wg%d" % i, [128, 16, 256], BF16) for i in range(NGU)]
    wd = [P.sb("wd%d" % i, [128, 8, 512], BF16) for i in range(NDN)]
    tg = [P.sb("tg%d" % i, [128, 512]) for i in range(2)]
    ts_ = [P.sb("ts%d" % i, [128, 512]) for i in range(2)]
    tu = [P.sb("tu%d" % i, [128, 512]) for i in range(2)]
    banks = [P.bank() for _ in range(8)]

    P.dma("sp", ident[:], id_d, writes=["ident"])
    P.dma("sp", nw_b[:], nw_d, writes=["nw_b"])
    P.dma("sp", wr[:], wr_d, writes=["wr"])
    P.dma("sp", br_b[:], br_d, writes=["br"])
    P.dma("sp", bgu[:], bgu_d, writes=["bgu"])
    P.dma("sp", bdn[:], bdn_d, writes=["bdn"])
    for s in range(2):
        P.dma("sp", G_b[s][:], gf_d[s], writes=["G%d" % s])

    cur_seg = [-1]

    def load_seg(s):
        if cur_seg[0] == s:
            return
        cur_seg[0] = s
        P.dma("sp", A_b[:], sc_d[s], writes=["A"])
        P.dma("sp", B_b[:], sh_d[s], writes=["B"])
        P.op("dve", lambda e: e.scalar_tensor_tensor(out=A_b[:], in0=A_b[:], scalar=1.0, in1=nw_b[:],
                                                     op0=ALU.add, op1=ALU.mult),
             reads=["A", "nw_b"], writes=["A"])

    wq = {"gu": 0, "dn": 0}

    def load_gu(e_, fc):
        i = wq["gu"] % NGU
        wq["gu"] += 1
        src = wgu_d[e_, :, fc * 256:(fc + 1) * 256].rearrange("(k p) c -> p k c", p=128)
        P.dma("pool", wg[i][:], src, writes=["wg%d" % i])
        return i

    def load_dn(e_, db):
        i = wq["dn"] % NDN
        wq["dn"] += 1
        src = wdn_d[e_, :, db * 512:(db + 1) * 512].rearrange("(k p) c -> p k c", p=128)
        P.dma("pool", wd[i][:], src, writes=["wd%d" % i])
        return i

    bk = {"gu": 0, "dn": 0, "misc": 0}

    for tiles in passes:
        npt = len(tiles)
        for j, t in enumerate(tiles):
            load_seg(0 if t < n_lat else 1)
            P.dma("sp", xt[:], x_d[t * 128:(t + 1) * 128, :], writes=["xt"])
            P.op("act", lambda e: e.activation(out=h32T[:].rearrange("p a b -> p (a b)"), in_=xt[:], func=AF.Square,
                                               accum_out=sm[:, 0:1]),
                 reads=["xt"], writes=["h32T", "sm0"])
            P.op("dve", lambda e: e.tensor_scalar(out=sm[:, 1:2], in0=sm[:, 0:1], scalar1=1.0 / D, scalar2=EPS,
                                                  op0=ALU.mult, op1=ALU.add), reads=["sm0"], writes=["sm1"])
            P.op("act", lambda e: e.activation(out=sm[:, 2:3], in_=sm[:, 1:2], func=AF.Sqrt),
                 reads=["sm1"], writes=["sm2"])
            P.op("dve", lambda e: e.reciprocal(out=sm[:, 3:4], in_=sm[:, 2:3]), reads=["sm2"], writes=["sm3"])
            P.op("dve", lambda e: e.scalar_tensor_tensor(out=xt[:], in0=xt[:], scalar=sm[:, 3:4], in1=A_b[:],
                                                         op0=ALU.mult, op1=ALU.mult),
                 reads=["xt", "sm3", "A"], writes=["xt"])
            P.op("dve", lambda e: e.tensor_tensor(out=xt[:], in0=xt[:], in1=B_b[:], op=ALU.add),
                 reads=["xt", "B"], writes=["xt"])
            for q in range(4):
                b = banks[4 + (bk["misc"] % 2)]
                bn = "bank%d" % (4 + (bk["misc"] % 2))
                bk["misc"] += 1

                def tr(e, q=q, b=b):
                    ins = None
                    for r in range(4):
                        k = q * 4 + r
                        ins = e.transpose(b[:, r * 128:(r + 1) * 128], xt[:, k * 128:(k + 1) * 128], ident[:])
                    return ins
                P.op("pe", tr, reads=["xt", "ident"], writes=[bn])
                P.op("act", lambda e, q=q, b=b: e.activation(
                    out=h32T[:, q * 4:(q + 1) * 4, :].rearrange("p a b -> p (a b)"), in_=b[:], func=AF.Identity),
                    reads=[bn], writes=["h32T"])
            P.op("pool", lambda e, j=j: e.tensor_copy(out=hT[:, :, j * 128:(j + 1) * 128], in_=h32T[:]),
                 reads=["h32T"], writes=["hT"])
            b = banks[6]

            def rt(e, b=b):
                ins = None
                for k in range(16):
                    ins = e.matmul(b[:, 0:ne], lhsT=h32T[:, k, :], rhs=wr[:, k, :], start=(k == 0), stop=(k == 15))
                return ins
            P.op("pe", rt, reads=["h32T", "wr"], writes=["bank6"])
            P.op("dve", lambda e, b=b: e.tensor_tensor(out=lg[:], in0=b[:, 0:ne], in1=br_b[:], op=ALU.add),
                 reads=["bank6", "br"], writes=["lg"])
            P.op("dve", lambda e: e.max(out=m8[:], in_=lg[:]), reads=["lg"], writes=["m8"])
            P.op("dve", lambda e: e.tensor_scalar(out=sm[:, 4:5], in0=m8[:, 0:1], scalar1=-1.0, scalar2=None,
                                                  op0=ALU.mult), reads=["m8"], writes=["sm4"])
            P.op("act", lambda e, j=j: e.activation(out=gates[:, j, :], in_=lg[:], func=AF.Exp, bias=sm[:, 4:5]),
                 reads=["lg", "sm4"], writes=["gates"])
            P.op("dve", lambda e: e.tensor_scalar(out=lg[:], in0=lg[:], scalar1=m8[:, 3:4], scalar2=None,
                                                  op0=ALU.is_ge), reads=["lg", "m8"], writes=["lg"])
            P.op("dve", lambda e, j=j: e.tensor_tensor(out=gates[:, j, :], in0=gates[:, j, :], in1=lg[:],
                                                       op=ALU.mult), reads=["gates", "lg"], writes=["gates"])
            P.op("dve", lambda e, j=j: e.reduce_sum(out=sm[:, 5:6], in_=gates[:, j, :],
                                                    axis=mybir.AxisListType.X), reads=["gates"], writes=["sm5"])
            P.op("dve", lambda e: e.reciprocal(out=sm[:, 6:7], in_=sm[:, 5:6]), reads=["sm5"], writes=["sm6"])
            P.op("dve", lambda e, j=j: e.tensor_scalar(out=gates[:, j, :], in0=gates[:, j, :], scalar1=sm[:, 6:7],
                                                       scalar2=None, op0=ALU.mult),
                 reads=["gates", "sm6"], writes=["gates"])
            b = banks[7]
            P.op("pe", lambda e, j=j, b=b: e.transpose(b[0:ne, 0:128], gates[:, j, :], ident[:]),
                 reads=["gates", "ident"], writes=["bank7"])
            P.op("act", lambda e, j=j, b=b: e.activation(out=gT[:, j * 128:(j + 1) * 128], in_=b[0:ne, 0:128],
                                                         func=AF.Identity), reads=["bank7"], writes=["gT"])
        for j in range(npt):
            for db in range(4):
                b = banks[4 + (bk["misc"] % 2)]
                bn = "bank%d" % (4 + (bk["misc"] % 2))
                bk["misc"] += 1
                P.op("pe", lambda e, j=j, db=db, b=b: e.matmul(b[:], lhsT=gT[:, j * 128:(j + 1) * 128],
                                                               rhs=bdn[:, db * 512:(db + 1) * 512],
                                                               start=True, stop=True),
                     reads=["gT", "bdn"], writes=[bn])
                P.op("act", lambda e, j=j, db=db, b=b: e.activation(out=acc[:, j, db * 512:(db + 1) * 512],
                                                                    in_=b[:], func=AF.Identity),
                     reads=[bn], writes=["acc%d" % j])
        if npt * 128 <= 512:
            blocks = [(0, npt * 128)]
        else:
            h1 = ((npt + 1) // 2) * 128
            blocks = [(0, h1), (h1, npt * 128)]
        units = []
        for e_ in range(ne):
            units += [("gu", e_, fc) for fc in range(8)] + [("dn", e_, db) for db in range(4)]
        loaded = {}

        def ensure(i):
            if i < len(units) and i not in loaded:
                kind, ee, ii = units[i]
                loaded[i] = load_gu(ee, ii) if kind == "gu" else load_dn(ee, ii)
        for ui, (kind, e_, idx) in enumerate(units):
            ensure(ui)
            ensure(ui + 1)
            ensure(ui + 2)
            wi = loaded.pop(ui)
            if kind == "gu":
                fc = idx
                for (c0, c1) in blocks:
                    n = c1 - c0
                    i2 = bk["gu"] % 2
                    bk["gu"] += 1
                    bg_, bu_ = banks[2 * i2], banks[2 * i2 + 1]
                    bgn, bun = "bank%d" % (2 * i2), "bank%d" % (2 * i2 + 1)

                    def mm(e, par, b):
                        ins = None
                        for k in range(16):
                            ins = e.matmul(b[:, 0:n], lhsT=wg[wi][:, k, par:256:2], rhs=hT[:, k, c0:c1],
                                           start=(k == 0), stop=(k == 15))
                        return ins
                    P.op("pe", lambda e: mm(e, 0, bg_), reads=["wg%d" % wi, "hT"], writes=[bgn])
                    P.op("pe", lambda e: mm(e, 1, bu_), reads=["wg%d" % wi, "hT"], writes=[bun])
                    g_, s_, u_ = tg[i2], ts_[i2], tu[i2]
                    gn, sn, un = "tg%d" % i2, "ts%d" % i2, "tu%d" % i2
                    P.op("dve", lambda e: e.tensor_scalar(out=g_[:, 0:n], in0=bg_[:, 0:n],
                                                          scalar1=bgu[:, e_, 0, fc:fc + 1], scalar2=7.0,
                                                          op0=ALU.add, op1=ALU.min),
                         reads=[bgn, "bgu"], writes=[gn])
                    P.op("act", lambda e: e.activation(out=s_[:, 0:n], in_=g_[:, 0:n], func=AF.Sigmoid, scale=1.702),
                         reads=[gn], writes=[sn])
                    P.op("dve", lambda e: e.tensor_scalar(out=u_[:, 0:n], in0=bu_[:, 0:n],
                                                          scalar1=bgu[:, e_, 1, fc:fc + 1], scalar2=-7.0,
                                                          op0=ALU.add, op1=ALU.max),
                         reads=[bun, "bgu"], writes=[un])
                    P.op("dve", lambda e: e.tensor_scalar(out=u_[:, 0:n], in0=u_[:, 0:n], scalar1=7.0, scalar2=1.0,
                                                          op0=ALU.min, op1=ALU.add), reads=[un], writes=[un])
                    P.op("dve", lambda e: e.tensor_tensor(out=g_[:, 0:n], in0=g_[:, 0:n], in1=s_[:, 0:n],
                                                          op=ALU.mult), reads=[gn, sn], writes=[gn])
                    P.op("dve", lambda e: e.tensor_tensor(out=actT[:, fc, c0:c1], in0=g_[:, 0:n], in1=u_[:, 0:n],
                                                          op=ALU.mult), reads=[gn, un], writes=["actT%d" % fc])
            else:
                db = idx
                for j in range(npt):
                    i3 = 4 + (bk["dn"] % 3)
                    bk["dn"] += 1
                    b = banks[i3]
                    bn = "bank%d" % i3

                    def dn(e):
                        ins = None
                        for fc2 in range(8):
                            ins = e.matmul(b[:], lhsT=actT[:, fc2, j * 128:(j + 1) * 128], rhs=wd[wi][:, fc2, :],
                                           start=(fc2 == 0), stop=(fc2 == 7))
                        return ins
                    P.op("pe", dn, reads=["wd%d" % wi] + ["actT%d" % f for f in range(8)], writes=[bn])
                    P.op("dve", lambda e: e.scalar_tensor_tensor(
                        out=acc[:, j, db * 512:(db + 1) * 512], in0=b[:], scalar=gates[:, j, e_:e_ + 1],
                        in1=acc[:, j, db * 512:(db + 1) * 512], op0=ALU.mult, op1=ALU.add),
                        reads=[bn, "gates", "acc%d" % j], writes=["acc%d" % j])
        for j, t in enumerate(tiles):
            s = 0 if t < n_lat else 1
            P.dma("sp", xt[:], x_d[t * 128:(t + 1) * 128, :], writes=["xt"])
            P.op("pool", lambda e, j=j, s=s: e.tensor_tensor(out=acc[:, j, :], in0=acc[:, j, :], in1=G_b[s][:],
                                                             op=ALU.mult), reads=["acc%d" % j, "G%d" % s],
                 writes=["acc%d" % j])
            P.op("pool", lambda e, j=j: e.tensor_tensor(out=acc[:, j, :], in0=acc[:, j, :], in1=xt[:], op=ALU.add),
                 reads=["acc%d" % j, "xt"], writes=["acc%d" % j])
            P.dma("sp", y_d[t * 128:(t + 1) * 128, :], acc[:, j, :], reads=["acc%d" % j])
    return P.finish()


def bcast(v):
    return np.ascontiguousarray(np.broadcast_to(np.asarray(v, np.float32)[None, :], (128, v.shape[-1])))


def moe_inputs(x_tiles, nw, sc, sh, gf, w_router, b_router, w_gu, b_gu, w_dn, b_dn):
    ne = w_router.shape[1]
    return {
        "x": np.ascontiguousarray(x_tiles, np.float32),
        "nw_b": bcast(nw),
        "sc_b": np.stack([bcast(sc[0]), bcast(sc[1])]),
        "sh_b": np.stack([bcast(sh[0]), bcast(sh[1])]),
        "gf_b": np.stack([bcast(gf[0]), bcast(gf[1])]),
        "wr": np.ascontiguousarray(w_router.reshape(16, 128, ne).transpose(1, 0, 2)),
        "br_b": bcast(b_router),
        "wgu": w_gu,
        "bgu": np.ascontiguousarray(b_gu.reshape(ne, 8, 128, 2).transpose(2, 0, 3, 1)),
        "wdn": w_dn,
        "bdn": b_dn,
        "ident": np.eye(128, dtype=np.float32),
    }


def build_mod():
    P = Prog()
    wm_d = P.dram_in("wm", [D, 6144])
    cT_d = P.dram_in("cT", [128, 16, 5])
    bm_d = P.dram_in("bmT", [128, 48])
    o_d = P.dram_out("modT", [128, 48, 5])
    cs = P.sb("cs", [128, 16, 5])
    bm = P.sb("bm", [128, 48])
    res = P.sb("res", [128, 48, 5])
    st = [P.sb("st%d" % i, [128, 16, 256]) for i in range(3)]
    banks = [P.bank() for _ in range(2)]
    P.dma("sp", cs[:], cT_d, writes=["cs"])
    P.dma("sp", bm[:], bm_d, writes=["bm"])
    P.op("act", lambda e: e.activation(out=cs[:].rearrange("p a b -> p (a b)"),
                                       in_=cs[:].rearrange("p a b -> p (a b)"), func=AF.Silu),
         reads=["cs"], writes=["cs"])
    for cb in range(24):
        s = st[cb % 3]
        sn = "st%d" % (cb % 3)
        P.dma("sp", s[:], wm_d[:, cb * 256:(cb + 1) * 256].rearrange("(k p) c -> p k c", p=128), writes=[sn])
        for dc in range(2):
            ch = cb * 2 + dc
            b = banks[ch % 2]
            bn = "bank%d" % (ch % 2)

            def mm(e, b=b, s=s, dc=dc):
                ins = None
                for k in range(16):
                    ins = e.matmul(b[:, 0:5], lhsT=s[:, k, dc * 128:(dc + 1) * 128], rhs=cs[:, k, :],
                                   start=(k == 0), stop=(k == 15))
                return ins
            P.op("pe", mm, reads=[sn, "cs"], writes=[bn])
            P.op("dve", lambda e, b=b, ch=ch: e.tensor_scalar(out=res[:, ch, :], in0=b[:, 0:5],
                                                              scalar1=bm[:, ch:ch + 1], scalar2=None, op0=ALU.add),
                 reads=[bn, "bm"], writes=["res"])
    P.dma("sp", o_d, res[:], reads=["res"])
    return P.finish()


def run_mod(c, c_ctx, w_mod, b_mod):
    nc = build_mod()
    rows = np.concatenate([c, c_ctx[None, :]], 0).astype(np.float32)
    cT = np.ascontiguousarray(rows.T.reshape(16, 128, 5).transpose(1, 0, 2))
    maps = []
    for core in range(8):
        l, hf = core // 2, core % 2
        maps.append({
            "wm": np.ascontiguousarray(w_mod[l][:, hf * 6144:(hf + 1) * 6144]),
            "cT": cT,
            "bmT": np.ascontiguousarray(b_mod[l][hf * 6144:(hf + 1) * 6144].reshape(48, 128).T),
        })
    res = run_bass_kernel_spmd(nc, maps, core_ids=list(range(8)))
    mod = np.zeros((4, 5, 12288), np.float32)
    for core in range(8):
        l, hf = core // 2, core % 2
        m = res.results[core]["modT"]
        mod[l, :, hf * 6144:(hf + 1) * 6144] = m.transpose(2, 1, 0).reshape(5, 6144)
    return mod


HD = 128
ATT_NT = 20


def build_attn():
    P = Prog()
    NTOK = ATT_NT * 128
    xT_d = P.dram_in("xT", [D, NTOK])
    nw_d = P.dram_in("nwT", [128, 16])
    sc_d = P.dram_in("scT", [128, 16, 2])
    sh_d = P.dram_in("shT", [128, 16, 2])
    g_d = P.dram_in("gT", [128, 16, 2])
    wqkv_d = P.dram_in("wqkv", [D, 3072])
    wo_d = P.dram_in("wo", [D, D])
    qg_d = P.dram_in("qg", [128, 1])
    kg_d = P.dram_in("kg", [128, 1])
    sink_d = P.dram_in("sinkrep", [1, 4, 512])
    cos_d = P.dram_in("cosrep", [18, 128, 512])
    sin_d = P.dram_in("sinrep", [18, 128, 512])
    rm_d = P.dram_in("rmat", [128, 128])
    mask_d = P.dram_in("masks", [4, 128, 512])
    id_d = P.dram_in("ident", [128, 128])
    o_d = P.dram_out("xoT", [D, 18 * 128])

    wq_s = P.sb("wq", [128, 16, 1024], BF16)
    wo_s = P.sb("wo", [128, 16, 2048], BF16)
    kT = P.sb("kT", [128, 4, NTOK], BF16)
    v_s = P.sb("v", [128, ATT_NT, 512], BF16)
    xt = P.sb("xt", [128, 16, 128])
    sq = P.sb("sq", [128, 16, 128])
    uT = P.sb("uT", [128, 16, 128], BF16)
    qT = P.sb("qT", [128, 16, 128], BF16)
    oT = P.sb("oT", [128, 16, 128], BF16)
    pT = P.sb("pT", [128, 5, 512], BF16)
    t1 = P.sb("t1", [128, 512])
    t2 = P.sb("t2", [128, 512])
    t3 = P.sb("t3", [128, 512])
    ct = P.sb("ct", [128, 512])
    st = P.sb("st", [128, 512])
    rsd = P.sb("rsd", [128, 128])
    nw = P.sb("nwT", [128, 16])
    A = P.sb("A", [128, 16, 2])
    B = P.sb("B", [128, 16, 2])
    G = P.sb("G", [128, 16, 2])
    qg = P.sb("qg", [128, 1])
    kg = P.sb("kg", [128, 1])
    sink32 = P.sb("sink32", [1, 4, 512])
    sinkb = P.sb("sinkb", [1, 4, 512], BF16)
    rmat = P.sb("rmat", [128, 128])
    m32 = P.sb("m32", [128, 512])
    maskb = P.sb("maskb", [128, 4, 512], BF16)
    id32 = P.sb("id32", [128, 128])
    idb = P.sb("idb", [128, 128], BF16)
    ones32 = P.sb("ones32", [128, 128])
    onesb = P.sb("onesb", [128, 128], BF16)
    banks = [P.bank() for _ in range(8)]
    bn = ["bank%d" % i for i in range(8)]

    for dst, src, nm in ((nw, nw_d, "nw"), (A, sc_d, "A"), (B, sh_d, "B"), (G, g_d, "G"), (qg, qg_d, "qg"),
                         (kg, kg_d, "kg"), (sink32, sink_d, "sink32"), (rmat, rm_d, "rmat"), (id32, id_d, "id32")):
        P.dma("sp", dst[:], src, writes=[nm])
    P.op("dve", lambda e: e.tensor_copy(out=idb[:], in_=id32[:]), reads=["id32"], writes=["idb"])
    P.op("dve", lambda e: e.memset(ones32[:], 1.0), writes=["ones32"])
    P.op("dve", lambda e: e.memset(onesb[:], 1.0), writes=["onesb"])
    P.op("act", lambda e: e.activation(out=sinkb[:].rearrange("p a b -> p (a b)"),
                                       in_=sink32[:].rearrange("p a b -> p (a b)"), func=AF.Exp),
         reads=["sink32"], writes=["sinkb"])
    for i in range(4):
        P.dma("sp", m32[:], mask_d[i], writes=["m32"])
        P.op("dve", lambda e, i=i: e.tensor_copy(out=maskb[:, i, :], in_=m32[:]), reads=["m32"], writes=["maskb"])
    for s in range(2):
        P.op("dve", lambda e, s=s: e.scalar_tensor_tensor(out=A[:, :, s], in0=A[:, :, s], scalar=1.0, in1=nw[:],
                                                          op0=ALU.add, op1=ALU.mult),
             reads=["A", "nw"], writes=["A"])
    P.dma("pool", wq_s[:, :, 0:512], wqkv_d[:, 2048:2560].rearrange("(k p) c -> p k c", p=128), writes=["wq"])
    P.dma("pool", wq_s[:, :, 512:1024], wqkv_d[:, 2560:3072].rearrange("(k p) c -> p k c", p=128), writes=["wq"])
    for hh in range(4):
        P.dma("pool", wo_s[:, hh * 4:(hh + 1) * 4, :],
              wo_d[hh * 512:(hh + 1) * 512, :].rearrange("(h p) c -> p h c", p=128), writes=["wo"])

    def prenorm(t, seg):
        c0 = t * 128
        P.dma("sp", xt[:], xT_d[:, c0:c0 + 128].rearrange("(k p) c -> p k c", p=128), writes=["xt"])
        P.op("act", lambda e: e.activation(out=sq[:].rearrange("p a b -> p (a b)"),
                                           in_=xt[:].rearrange("p a b -> p (a b)"), func=AF.Square),
             reads=["xt"], writes=["sq"])

        def mm(e):
            ins = None
            for k in range(16):
                ins = e.matmul(banks[7][:, 0:128], lhsT=ones32[:], rhs=sq[:, k, :], start=(k == 0), stop=(k == 15))
            return ins
        P.op("pe", mm, reads=["sq", "ones32"], writes=[bn[7]])
        P.op("act", lambda e: e.activation(out=rsd[:], in_=banks[7][:, 0:128], func=AF.Sqrt, scale=1.0 / D, bias=EPS),
             reads=[bn[7]], writes=["rsd"])
        P.op("dve", lambda e: e.reciprocal(out=rsd[:], in_=rsd[:]), reads=["rsd"], writes=["rsd"])
        for k in range(16):
            P.op("dve", lambda e, k=k: e.scalar_tensor_tensor(out=sq[:, k, :], in0=xt[:, k, :],
                                                              scalar=A[:, k, seg:seg + 1], in1=rsd[:],
                                                              op0=ALU.mult, op1=ALU.mult),
                 reads=["xt", "A", "rsd"], writes=["sq"])
            P.op("act", lambda e, k=k: e.activation(out=uT[:, k, :], in_=sq[:, k, :], func=AF.Identity,
                                                    bias=B[:, k, seg:seg + 1]),
                 reads=["sq", "B"], writes=["uT"])

    def norm_rope(src_bank, src_name, gain, rope_tile, dst3, dst_name):
        P.op("act", lambda e: e.activation(out=t1[:], in_=src_bank[:], func=AF.Square), reads=[src_name], writes=["t1"])
        P.op("pe", lambda e: e.matmul(banks[7][:], lhsT=ones32[:], rhs=t1[:], start=True, stop=True),
             reads=["t1", "ones32"], writes=[bn[7]])
        P.op("act", lambda e: e.activation(out=t2[:], in_=banks[7][:], func=AF.Sqrt, scale=1.0 / HD, bias=EPS),
             reads=[bn[7]], writes=["t2"])
        P.op("dve", lambda e: e.reciprocal(out=t2[:], in_=t2[:]), reads=["t2"], writes=["t2"])
        dst2 = dst3
        if rope_tile is None:
            P.op("dve", lambda e: e.scalar_tensor_tensor(out=dst2, in0=src_bank[:].rearrange("p (a b) -> p a b", a=4),
                                                         scalar=gain[:, 0:1],
                                                         in1=t2[:].rearrange("p (a b) -> p a b", a=4),
                                                         op0=ALU.mult, op1=ALU.mult),
                 reads=[src_name, "t2", "qg", "kg"], writes=[dst_name])
            return
        P.op("dve", lambda e: e.scalar_tensor_tensor(out=t1[:], in0=src_bank[:], scalar=gain[:, 0:1], in1=t2[:],
                                                     op0=ALU.mult, op1=ALU.mult),
             reads=[src_name, "t2", "qg", "kg"], writes=["t1"])
        P.dma("sp", ct[:], cos_d[rope_tile], writes=["ct"])
        P.dma("sp", st[:], sin_d[rope_tile], writes=["st"])
        P.op("pe", lambda e: e.matmul(banks[7][:], lhsT=rmat[:], rhs=t1[:], start=True, stop=True),
             reads=["t1", "rmat"], writes=[bn[7]])
        P.op("dve", lambda e: e.tensor_tensor(out=t3[:], in0=banks[7][:], in1=st[:], op=ALU.mult),
             reads=[bn[7], "st"], writes=["t3"])
        P.op("dve", lambda e: e.tensor_tensor(out=t1[:], in0=t1[:], in1=ct[:], op=ALU.mult),
             reads=["t1", "ct"], writes=["t1"])
        P.op("dve", lambda e: e.tensor_tensor(out=dst2, in0=t1[:].rearrange("p (a b) -> p a b", a=4),
                                              in1=t3[:].rearrange("p (a b) -> p a b", a=4), op=ALU.add),
             reads=["t1", "t3"], writes=[dst_name])

    for t in range(ATT_NT):
        seg = 0 if t < 18 else 1
        prenorm(t, seg)

        def kmm(e):
            ins = None
            for g in range(4):
                for k in range(16):
                    ins = e.matmul(banks[0][:, g * 128:(g + 1) * 128], lhsT=wq_s[:, k, g * 128:(g + 1) * 128],
                                   rhs=uT[:, k, :], start=(k == 0), stop=(k == 15))
            return ins
        P.op("pe", kmm, reads=["wq", "uT"], writes=[bn[0]])

        def vmm(e):
            ins = None
            for k in range(16):
                ins = e.matmul(banks[1][:], lhsT=uT[:, k, :], rhs=wq_s[:, k, 512:1024], start=(k == 0), stop=(k == 15))
            return ins
        P.op("pe", vmm, reads=["wq", "uT"], writes=[bn[1]])
        P.op("act", lambda e, t=t: e.activation(out=v_s[:, t, :], in_=banks[1][:], func=AF.Identity),
             reads=[bn[1]], writes=["v"])
        norm_rope(banks[0], bn[0], kg, t if t < 18 else None, kT[:, :, t * 128:(t + 1) * 128], "kT")

    scale = HD ** -0.5
    for qi, t in enumerate(list(range(1, 17)) + [18, 19]):
        seg = 0 if t < 18 else 1
        prenorm(t, seg)
        for hh in range(2):
            P.dma("pool", wq_s[:], wqkv_d[:, hh * 1024:(hh + 1) * 1024].rearrange("(k p) c -> p k c", p=128),
                  writes=["wq"])
            for gg in range(2):
                b = banks[gg]

                def qmm(e, b=b, gg=gg):
                    ins = None
                    for h in range(4):
                        for k in range(16):
                            c = (gg * 4 + h) * 128
                            ins = e.matmul(b[:, h * 128:(h + 1) * 128], lhsT=wq_s[:, k, c:c + 128], rhs=uT[:, k, :],
                                           start=(k == 0), stop=(k == 15))
                    return ins
                P.op("pe", qmm, reads=["wq", "uT"], writes=[bn[gg]])
                g4 = (hh * 2 + gg) * 4
                norm_rope(b, bn[gg], qg, t if t < 18 else None, qT[:, g4:g4 + 4, :], "qT")
        if t < 18:
            kbs = [(t - 1, 2 if t == 1 else 0), (t, None), (t + 1, 3 if t == 16 else 1), (18, None), (19, None)]
        else:
            kbs = [(18, None), (19, None)]
        for g in range(4):
            q2 = qT[:, g * 4:(g + 1) * 4, :].rearrange("p a b -> p (a b)")
            for i, (kt, mk) in enumerate(kbs):
                sb_ = banks[2 + (i % 2)]
                sbn = bn[2 + (i % 2)]

                def smm(e, sb_=sb_, kt=kt, mk=mk):
                    ins = e.matmul(sb_[:], lhsT=kT[:, g, kt * 128:(kt + 1) * 128], rhs=q2, start=True, stop=(mk is None))
                    if mk is not None:
                        ins = e.matmul(sb_[:], lhsT=idb[:], rhs=maskb[:, mk, :], start=False, stop=True)
                    return ins
                P.op("pe", smm, reads=["kT", "qT", "idb", "maskb"], writes=[sbn])
                P.op("act", lambda e, sb_=sb_, i=i: e.activation(out=pT[:, i, :], in_=sb_[:], func=AF.Exp, scale=scale),
                     reads=[sbn], writes=["pT%d" % i])

            def omm(e):
                ins = None
                for i, (kt, mk) in enumerate(kbs):
                    ins = e.matmul(banks[4][:], lhsT=v_s[:, kt, g * 128:(g + 1) * 128], rhs=pT[:, i, :],
                                   start=(i == 0), stop=(i == len(kbs) - 1))
                return ins
            P.op("pe", omm, reads=["v"] + ["pT%d" % i for i in range(len(kbs))], writes=[bn[4]])

            def dmm(e):
                for i in range(len(kbs)):
                    e.matmul(banks[5][:], lhsT=onesb[:], rhs=pT[:, i, :], start=(i == 0), stop=False)
                return e.matmul(banks[5][:], lhsT=onesb[0:1, :], rhs=sinkb[0:1, g, :], start=False, stop=True)
            P.op("pe", dmm, reads=["onesb", "sinkb"] + ["pT%d" % i for i in range(len(kbs))], writes=[bn[5]])
            P.op("dve", lambda e: e.reciprocal(out=t2[:], in_=banks[5][:]), reads=[bn[5]], writes=["t2"])
            P.op("dve", lambda e, g=g: e.tensor_tensor(out=oT[:, g * 4:(g + 1) * 4, :],
                                                       in0=banks[4][:].rearrange("p (a b) -> p a b", a=4),
                                                       in1=t2[:].rearrange("p (a b) -> p a b", a=4), op=ALU.mult),
                 reads=[bn[4], "t2"], writes=["oT"])
        for dc in range(16):
            b = banks[6 + (dc % 2)]

            def pmm(e, b=b, dc=dc):
                ins = None
                for h in range(16):
                    ins = e.matmul(b[:, 0:128], lhsT=wo_s[:, h, dc * 128:(dc + 1) * 128], rhs=oT[:, h, :],
                                   start=(h == 0), stop=(h == 15))
                return ins
            P.op("pe", pmm, reads=["wo", "oT"], writes=[bn[6 + (dc % 2)]])
            P.op("dve", lambda e, b=b, dc=dc: e.scalar_tensor_tensor(out=sq[:, dc, :], in0=b[:, 0:128],
                                                                     scalar=G[:, dc, seg:seg + 1], in1=xt[:, dc, :],
                                                                     op0=ALU.mult, op1=ALU.add),
                 reads=[bn[6 + (dc % 2)], "G", "xt"], writes=["sq"])
        P.dma("sp", o_d[:, qi * 128:(qi + 1) * 128].rearrange("(k p) c -> p k c", p=128), sq[:], reads=["sq"])
    return P.finish()


def rope_tables(half):
    inv = 10000.0 ** (-np.arange(0, 64, 2, dtype=np.float64) / 64)
    tpos = np.arange(-128, 2048 + 128) + half * 2048
    rows, cols = tpos // 64, tpos % 64
    ang = np.concatenate([rows[:, None] * inv, rows[:, None] * inv, cols[:, None] * inv, cols[:, None] * inv], 1)
    cos = np.cos(ang).T.astype(np.float32)
    sin = np.sin(ang).T.astype(np.float32)

    def rep(tb):
        tb = tb.reshape(128, 18, 1, 128).transpose(1, 0, 2, 3)
        return np.ascontiguousarray(np.broadcast_to(tb, (18, 128, 4, 128)).reshape(18, 128, 512))
    return rep(cos), rep(sin)


def attn_consts(half):
    rm = np.zeros((128, 128), np.float32)
    for base in (0, 64):
        for p in range(32):
            rm[base + p + 32, base + p] = -1.0
            rm[base + p, base + p + 32] = 1.0
    j = np.arange(128)[:, None]
    i = np.arange(128)[None, :]
    NEG = -30000.0
    mp = np.where(j >= i, 0.0, NEG).astype(np.float32)
    mn = np.where(j <= i, 0.0, NEG).astype(np.float32)
    full = np.full((128, 128), NEG, np.float32)
    mp0 = full if half == 0 else mp
    mn15 = full if half == 1 else mn
    masks = np.stack([np.tile(m, (1, 4)) for m in (mp, mn, mp0, mn15)])
    cos, sin = rope_tables(half)
    return {"rmat": rm, "masks": np.ascontiguousarray(masks), "cosrep": cos, "sinrep": sin,
            "ident": np.eye(128, dtype=np.float32)}


def fm(v):
    return np.ascontiguousarray(np.asarray(v, np.float32).reshape(16, 128).T)


def fm2(v0, v1):
    return np.ascontiguousarray(np.stack([fm(v0), fm(v1)], -1))


def attn_inputs(xb, ctxb, half, nw, sh_l, sc_l, g_l, sh_c, sc_c, g_c, wqkv, wo, qg, kg, sinks):
    z = np.zeros((128, D), np.float32)
    prev = xb[half * 2048 - 128:half * 2048] if half == 1 else z
    nxt = xb[2048:2176] if half == 0 else z
    cols = np.concatenate([prev, xb[half * 2048:(half + 1) * 2048], nxt, ctxb], 0)
    d = {"xT": np.ascontiguousarray(cols.T), "nwT": fm(nw), "scT": fm2(sc_l, sc_c), "shT": fm2(sh_l, sh_c),
         "gT": fm2(g_l, g_c), "wqkv": wqkv, "wo": wo, "qg": np.ascontiguousarray(qg.reshape(128, 1)),
         "kg": np.ascontiguousarray(kg.reshape(128, 1)),
         "sinkrep": np.ascontiguousarray(np.repeat(np.asarray(sinks, np.float32), 128).reshape(1, 4, 512))}
    d.update(attn_consts(half))
    return d


I32 = mybir.dt.int32
S5_NTOK = 4352
TWO_PI = float(2 * np.pi)


def build_s5():
    P = Prog()
    NT = S5_NTOK // 128
    xT_d = P.dram_in("xT", [D, S5_NTOK])
    nw_d = P.dram_in("nwT", [128, 16])
    sc_d = P.dram_in("scT", [128, 16, 2])
    sh_d = P.dram_in("shT", [128, 16, 2])
    are_d = P.dram_in("are", [128, 64])
    aim_d = P.dram_in("aim", [128, 64])
    ldt_d = P.dram_in("ldt", [128, 64])
    bre_d = P.dram_in("bre", [64, 128, 128])
    bim_d = P.dram_in("bim", [64, 128, 128])
    cre_d = P.dram_in("cre", [64, 128, 128])
    cim_d = P.dram_in("cim", [64, 128, 128])
    iota_d = P.dram_in("iota", [128, 512])
    y_d = P.dram_out("yT", [D, S5_NTOK])

    rstd = P.sb("rstd", [128, S5_NTOK])
    uk = P.sb("uk", [128, S5_NTOK])
    xt = P.sb("xt", [128, 16, 128])
    sq = P.sb("sq", [128, 16, 128])
    ones32 = P.sb("ones32", [128, 128])
    iota = P.sb("iota", [128, 512])
    nw = P.sb("nwT", [128, 16])
    A = P.sb("A", [128, 16, 2])
    B = P.sb("B", [128, 16, 2])
    prm = {n: P.sb("p_" + n, [128, 64]) for n in
           ("are", "aim", "dt", "th", "r", "c1", "s1", "cr", "ci", "cT", "sT", "ta", "tb", "tc", "kf")}
    ki64 = P.sb("ki64", [128, 64], I32)
    tabs = [{n: P.sb("tab_%s%d" % (n, i), [128, 512]) for n in ("Wr", "Wi", "cos", "sin", "R")} for i in range(4)]
    wts = [{n: P.sb("w_%s%d" % (n, i), [128, 128]) for n in ("bre", "bim", "cre", "ncre", "ncim")} for i in range(4)]
    carry = [P.sb("carry%d" % i, [128, 4]) for i in range(4)]
    ki = P.sb("ki", [128, 512], I32)
    kf = P.sb("kf", [128, 512])
    W = {n: P.sb("wk_" + n, [128, 512]) for n in ("m1", "m2", "m3", "m4", "gr", "gi", "Gr", "Gi", "p1", "p2", "p3", "p4")}
    yo = [P.sb("yo%d" % i, [128, 512]) for i in range(2)]
    banks = [P.bank() for _ in range(8)]
    bn = ["bank%d" % i for i in range(8)]

    for dst, src, nm in ((nw, nw_d, "nw"), (A, sc_d, "A"), (B, sh_d, "B"), (iota, iota_d, "iota"),
                         (prm["are"], are_d, "are"), (prm["aim"], aim_d, "aim"), (prm["dt"], ldt_d, "dt")):
        P.dma("sp", dst[:], src, writes=[nm])
    P.op("dve", lambda e: e.memset(ones32[:], 1.0), writes=["ones32"])
    for s in range(2):
        P.op("dve", lambda e, s=s: e.scalar_tensor_tensor(out=A[:, :, s], in0=A[:, :, s], scalar=1.0, in1=nw[:],
                                                          op0=ALU.add, op1=ALU.mult), reads=["A", "nw"], writes=["A"])

    def ts(out, in0, s1, op0, s2=None, op1=None, rd=(), wr=()):
        if op1 is None:
            P.op("dve", lambda e: e.tensor_scalar(out=out, in0=in0, scalar1=s1, scalar2=None, op0=op0), reads=rd, writes=wr)
        else:
            P.op("dve", lambda e: e.tensor_scalar(out=out, in0=in0, scalar1=s1, scalar2=s2, op0=op0, op1=op1),
                 reads=rd, writes=wr)

    def tt(out, a, b, op, rd=(), wr=(), eng="dve"):
        P.op(eng, lambda e: e.tensor_tensor(out=out, in0=a, in1=b, op=op), reads=rd, writes=wr)

    def rr_sin(dst, dname, x, xname, kI, kF, kname, shift=0.0):
        if shift != 0.0:
            ts(x, x, shift, ALU.add, rd=[xname], wr=[xname])
        ts(kI, x, 1.0 / TWO_PI, ALU.mult, rd=[xname], wr=[kname + "i"])
        P.op("dve", lambda e: e.tensor_copy(out=kF, in_=kI), reads=[kname + "i"], writes=[kname + "f"])
        P.op("dve", lambda e: e.scalar_tensor_tensor(out=x, in0=kF, scalar=-TWO_PI, in1=x, op0=ALU.mult, op1=ALU.add),
             reads=[kname + "f", xname], writes=[xname])
        ts(x, x, 3.14159, ALU.min, -3.14159, ALU.max, rd=[xname], wr=[xname])
        P.op("act", lambda e: e.activation(out=dst, in_=x, func=AF.Sin), reads=[xname], writes=[dname])

    p = prm
    P.op("act", lambda e: e.activation(out=p["dt"][:], in_=p["dt"][:], func=AF.Exp), reads=["dt"], writes=["dt"])
    ts(p["are"][:], p["are"][:], -1e-4, ALU.min, rd=["are"], wr=["are"])
    tt(p["ta"][:], p["are"][:], p["dt"][:], ALU.mult, rd=["are", "dt"], wr=["ta"])
    P.op("act", lambda e: e.activation(out=p["r"][:], in_=p["ta"][:], func=AF.Exp), reads=["ta"], writes=["r"])
    tt(p["th"][:], p["aim"][:], p["dt"][:], ALU.mult, rd=["aim", "dt"], wr=["th"])
    ts(ki64[:], p["th"][:], 1.0 / TWO_PI, ALU.mult, rd=["th"], wr=["ki64"])
    P.op("dve", lambda e: e.tensor_copy(out=p["kf"][:], in_=ki64[:]), reads=["ki64"], writes=["kf64"])
    P.op("dve", lambda e: e.scalar_tensor_tensor(out=p["th"][:], in0=p["kf"][:], scalar=-TWO_PI, in1=p["th"][:],
                                                 op0=ALU.mult, op1=ALU.add), reads=["kf64", "th"], writes=["th"])
    P.op("dve", lambda e: e.tensor_copy(out=p["ta"][:], in_=p["th"][:]), reads=["th"], writes=["ta"])
    rr_sin(p["s1"][:], "s1", p["ta"][:], "ta", ki64[:], p["kf"][:], "k64")
    P.op("dve", lambda e: e.tensor_copy(out=p["ta"][:], in_=p["th"][:]), reads=["th", "s1"], writes=["ta"])
    rr_sin(p["c1"][:], "c1", p["ta"][:], "ta", ki64[:], p["kf"][:], "k64", shift=float(np.pi / 2))
    ts(p["ta"][:], p["th"][:], 512.0, ALU.mult, rd=["th", "c1"], wr=["ta"])
    rr_sin(p["sT"][:], "sT", p["ta"][:], "ta", ki64[:], p["kf"][:], "k64")
    ts(p["ta"][:], p["th"][:], 512.0, ALU.mult, rd=["th", "sT"], wr=["ta"])
    rr_sin(p["cT"][:], "cT", p["ta"][:], "ta", ki64[:], p["kf"][:], "k64", shift=float(np.pi / 2))
    tt(p["ta"][:], p["r"][:], p["c1"][:], ALU.mult, rd=["r", "c1", "cT"], wr=["ta"])
    ts(p["ta"][:], p["ta"][:], -1.0, ALU.add, rd=["ta"], wr=["ta"])
    tt(p["tb"][:], p["r"][:], p["s1"][:], ALU.mult, rd=["r", "s1"], wr=["tb"])
    tt(p["tc"][:], p["are"][:], p["are"][:], ALU.mult, rd=["are"], wr=["tc"])
    tt(p["kf"][:], p["aim"][:], p["aim"][:], ALU.mult, rd=["aim", "cT"], wr=["kf64"])
    tt(p["tc"][:], p["tc"][:], p["kf"][:], ALU.add, rd=["tc", "kf64"], wr=["tc"])
    P.op("dve", lambda e: e.reciprocal(out=p["tc"][:], in_=p["tc"][:]), reads=["tc"], writes=["tc"])
    tt(p["cr"][:], p["ta"][:], p["are"][:], ALU.mult, rd=["ta", "are"], wr=["cr"])
    tt(p["kf"][:], p["tb"][:], p["aim"][:], ALU.mult, rd=["tb", "aim"], wr=["kf64"])
    tt(p["cr"][:], p["cr"][:], p["kf"][:], ALU.add, rd=["cr", "kf64"], wr=["cr"])
    tt(p["cr"][:], p["cr"][:], p["tc"][:], ALU.mult, rd=["cr", "tc"], wr=["cr"])
    tt(p["ci"][:], p["tb"][:], p["are"][:], ALU.mult, rd=["tb", "are"], wr=["ci"])
    tt(p["kf"][:], p["ta"][:], p["aim"][:], ALU.mult, rd=["ta", "aim", "cr"], wr=["kf64"])
    tt(p["ci"][:], p["ci"][:], p["kf"][:], ALU.subtract, rd=["ci", "kf64"], wr=["ci"])
    tt(p["ci"][:], p["ci"][:], p["tc"][:], ALU.mult, rd=["ci", "tc"], wr=["ci"])
    PRM = ["th", "r", "cr", "ci", "cT", "sT"]

    for t in range(NT):
        c0 = t * 128
        P.dma("sp", xt[:], xT_d[:, c0:c0 + 128].rearrange("(k p) c -> p k c", p=128), writes=["xt"])
        P.op("act", lambda e: e.activation(out=sq[:].rearrange("p a b -> p (a b)"),
                                           in_=xt[:].rearrange("p a b -> p (a b)"), func=AF.Square),
             reads=["xt"], writes=["sq"])
        b = banks[t % 2]

        def mm(e, b=b):
            ins = None
            for k in range(16):
                ins = e.matmul(b[:, 0:128], lhsT=ones32[:], rhs=sq[:, k, :], start=(k == 0), stop=(k == 15))
            return ins
        P.op("pe", mm, reads=["sq", "ones32"], writes=[bn[t % 2]])
        P.op("act", lambda e, b=b, c0=c0: e.activation(out=rstd[:, c0:c0 + 128], in_=b[:, 0:128], func=AF.Sqrt,
                                                       scale=1.0 / D, bias=EPS), reads=[bn[t % 2]], writes=["rstd"])
    P.op("dve", lambda e: e.reciprocal(out=rstd[:], in_=rstd[:]), reads=["rstd"], writes=["rstd"])

    chunks = [(c, min(c + 512, S5_NTOK)) for c in range(0, S5_NTOK, 512)]
    yi = 0
    for k in range(16):
        P.dma("sp", uk[:], xT_d[k * 128:(k + 1) * 128, :], writes=["uk"])
        tt(uk[:], uk[:], rstd[:], ALU.mult, rd=["uk", "rstd"], wr=["uk"])
        for (a0, a1, seg) in ((0, 256, 1), (256, S5_NTOK, 0)):
            ts(uk[:, a0:a1], uk[:, a0:a1], A[:, k, seg:seg + 1], ALU.mult, B[:, k, seg:seg + 1], ALU.add,
               rd=["uk", "A", "B"], wr=["uk"])
        for j in range(4):
            q = 4 * k + j
            T_, Wt = tabs[j], wts[j]
            tn = "tab%d" % j
            for nm, src in (("bre", bre_d), ("bim", bim_d), ("cre", cre_d), ("ncim", cim_d)):
                P.dma("sp", Wt[nm][:], src[q], writes=["w%d" % j])
            ts(Wt["ncre"][:], Wt["cre"][:], -1.0, ALU.mult, rd=["w%d" % j], wr=["w%d" % j])
            ts(Wt["ncim"][:], Wt["ncim"][:], -1.0, ALU.mult, rd=["w%d" % j], wr=["w%d" % j])
            thq = p["th"][:, q:q + 1]
            ts(W["m1"][:], iota[:], thq, ALU.mult, rd=["iota"] + PRM, wr=["m1"])
            rr_sin(T_["sin"][:], tn, W["m1"][:], "m1", ki[:], kf[:], "kk")
            ts(W["m1"][:], iota[:], thq, ALU.mult, rd=["iota", tn], wr=["m1"])
            rr_sin(T_["cos"][:], tn, W["m1"][:], "m1", ki[:], kf[:], "kk", shift=float(np.pi / 2))
            ts(W["m2"][:], T_["sin"][:], p["ci"][:, q:q + 1], ALU.mult, rd=[tn], wr=["m2"])
            P.op("dve", lambda e, T_=T_, q=q: e.scalar_tensor_tensor(out=T_["Wr"][:], in0=T_["cos"][:],
                                                                     scalar=p["cr"][:, q:q + 1], in1=W["m2"][:],
                                                                     op0=ALU.mult, op1=ALU.add),
                 reads=[tn, "m2"], writes=[tn])
            ts(W["m2"][:], T_["sin"][:], p["cr"][:, q:q + 1], ALU.mult, rd=[tn], wr=["m2"])
            P.op("dve", lambda e, T_=T_, q=q: e.scalar_tensor_tensor(out=T_["Wi"][:], in0=T_["cos"][:],
                                                                     scalar=p["ci"][:, q:q + 1], in1=W["m2"][:],
                                                                     op0=ALU.mult, op1=ALU.subtract),
                 reads=[tn, "m2"], writes=[tn])
            ts(T_["R"][:], iota[:], 0.0, ALU.mult, p["r"][:, q:q + 1], ALU.add, rd=["iota", tn], wr=[tn])
            P.op("dve", lambda e, j=j: e.memset(carry[j][:], 0.0), writes=["carry%d" % j])
        for (c0, c1) in chunks:
            n = c1 - c0
            yb = banks[2 + (yi % 2)]
            ybn = bn[2 + (yi % 2)]
            for j in range(4):
                q = 4 * k + j
                T_, Wt = tabs[j], wts[j]
                tn, wn, cn = "tab%d" % j, "w%d" % j, "carry%d" % j
                P.op("pe", lambda e, Wt=Wt: e.matmul(banks[4][:, 0:n], lhsT=Wt["bre"][:], rhs=uk[:, c0:c1],
                                                     start=True, stop=True), reads=[wn, "uk"], writes=[bn[4]])
                P.op("pe", lambda e, Wt=Wt: e.matmul(banks[5][:, 0:n], lhsT=Wt["bim"][:], rhs=uk[:, c0:c1],
                                                     start=True, stop=True), reads=[wn, "uk"], writes=[bn[5]])
                Pr, Pi = banks[4][:, 0:n], banks[5][:, 0:n]
                tt(W["m1"][:, 0:n], Pr, T_["Wr"][:, 0:n], ALU.mult, rd=[bn[4], tn], wr=["m1"])
                tt(W["m2"][:, 0:n], Pi, T_["Wi"][:, 0:n], ALU.mult, rd=[bn[5], tn], wr=["m2"])
                tt(W["m3"][:, 0:n], Pi, T_["Wr"][:, 0:n], ALU.mult, rd=[bn[5], tn], wr=["m3"])
                tt(W["m4"][:, 0:n], Pr, T_["Wi"][:, 0:n], ALU.mult, rd=[bn[4], tn], wr=["m4"])
                tt(W["gr"][:, 0:n], W["m1"][:, 0:n], W["m2"][:, 0:n], ALU.subtract, rd=["m1", "m2"], wr=["gr"], eng="pool")
                tt(W["gi"][:, 0:n], W["m3"][:, 0:n], W["m4"][:, 0:n], ALU.add, rd=["m3", "m4"], wr=["gi"], eng="pool")
                P.op("dve", lambda e, T_=T_, j=j: e.tensor_tensor_scan(out=W["Gr"][:, 0:n], data0=T_["R"][:, 0:n],
                                                                      data1=W["gr"][:, 0:n], initial=carry[j][:, 0:1],
                                                                      op0=ALU.mult, op1=ALU.add),
                     reads=[tn, "gr", cn], writes=["Gr"])
                P.op("dve", lambda e, T_=T_, j=j: e.tensor_tensor_scan(out=W["Gi"][:, 0:n], data0=T_["R"][:, 0:n],
                                                                      data1=W["gi"][:, 0:n], initial=carry[j][:, 1:2],
                                                                      op0=ALU.mult, op1=ALU.add),
                     reads=[tn, "gi", cn], writes=["Gi"])
                if n == 512:
                    gl_r, gl_i = W["Gr"][:, 511:512], W["Gi"][:, 511:512]
                    ts(carry[j][:, 2:3], gl_i, p["sT"][:, q:q + 1], ALU.mult, rd=["Gi"] + PRM, wr=[cn])
                    P.op("dve", lambda e, j=j, q=q: e.scalar_tensor_tensor(out=carry[j][:, 0:1], in0=gl_r,
                                                                           scalar=p["cT"][:, q:q + 1],
                                                                           in1=carry[j][:, 2:3],
                                                                           op0=ALU.mult, op1=ALU.subtract),
                         reads=["Gr", cn], writes=[cn])
                    ts(carry[j][:, 3:4], gl_i, p["cT"][:, q:q + 1], ALU.mult, rd=["Gi"], wr=[cn])
                    P.op("dve", lambda e, j=j, q=q: e.scalar_tensor_tensor(out=carry[j][:, 1:2], in0=gl_r,
                                                                           scalar=p["sT"][:, q:q + 1],
                                                                           in1=carry[j][:, 3:4],
                                                                           op0=ALU.mult, op1=ALU.add),
                         reads=["Gr", cn], writes=[cn])
                tt(W["p1"][:, 0:n], W["Gr"][:, 0:n], T_["cos"][:, 0:n], ALU.mult, rd=["Gr", tn], wr=["p1"])
                tt(W["p2"][:, 0:n], W["Gi"][:, 0:n], T_["sin"][:, 0:n], ALU.mult, rd=["Gi", tn], wr=["p2"])
                tt(W["p3"][:, 0:n], W["Gr"][:, 0:n], T_["sin"][:, 0:n], ALU.mult, rd=["Gr", tn], wr=["p3"], eng="pool")
                tt(W["p4"][:, 0:n], W["Gi"][:, 0:n], T_["cos"][:, 0:n], ALU.mult, rd=["Gi", tn], wr=["p4"], eng="pool")

                def cmm(e, Wt=Wt, j=j):
                    e.matmul(yb[:, 0:n], lhsT=Wt["cre"][:], rhs=W["p1"][:, 0:n], start=(j == 0), stop=False)
                    e.matmul(yb[:, 0:n], lhsT=Wt["ncre"][:], rhs=W["p2"][:, 0:n], start=False, stop=False)
                    e.matmul(yb[:, 0:n], lhsT=Wt["ncim"][:], rhs=W["p3"][:, 0:n], start=False, stop=False)
                    return e.matmul(yb[:, 0:n], lhsT=Wt["ncim"][:], rhs=W["p4"][:, 0:n], start=False, stop=(j == 3))
                P.op("pe", cmm, reads=[wn, "p1", "p2", "p3", "p4"], writes=[ybn])
            yo_ = yo[yi % 2]
            P.op("act", lambda e, yo_=yo_: e.activation(out=yo_[:, 0:n], in_=yb[:, 0:n], func=AF.Identity),
                 reads=[ybn], writes=["yo%d" % (yi % 2)])
            P.dma("sp", y_d[k * 128:(k + 1) * 128, c0:c1], yo_[:, 0:n], reads=["yo%d" % (yi % 2)])
            yi += 1
    return P.finish()


def s5_inputs(xT_scan, nw, sh_l, sc_l, sh_c, sc_c, a_re, a_im, log_dt, b_re, b_im, c_re, c_im):
    def pl(a):
        return np.ascontiguousarray(a.reshape(64, 2, 64).transpose(1, 2, 0).reshape(128, 64))
    ldt = np.broadcast_to(log_dt[:, None], (128, 64))
    Bre = np.zeros((64, 128, 128), np.float32)
    Bim = np.zeros_like(Bre)
    Cre = np.zeros_like(Bre)
    Cim = np.zeros_like(Bre)
    for q in range(64):
        for gi in range(2):
            g = 2 * q + gi
            r0 = (q % 4) * 32 + gi * 16
            Bre[q, r0:r0 + 16, gi * 64:(gi + 1) * 64] = b_re[g].T
            Bim[q, r0:r0 + 16, gi * 64:(gi + 1) * 64] = b_im[g].T
            Cre[q, gi * 64:(gi + 1) * 64, r0:r0 + 16] = c_re[g].T
            Cim[q, gi * 64:(gi + 1) * 64, r0:r0 + 16] = c_im[g].T
    return {"xT": xT_scan, "nwT": fm(nw), "scT": fm2(sc_l, sc_c), "shT": fm2(sh_l, sh_c),
            "are": pl(a_re), "aim": pl(a_im), "ldt": pl(ldt), "bre": Bre, "bim": Bim, "cre": Cre, "cim": Cim,
            "iota": np.ascontiguousarray(np.broadcast_to(np.arange(512, dtype=np.float32)[None, :], (128, 512)))}


def build_glu():
    P = Prog()
    NTOK = 18 * 128
    xT_d = P.dram_in("xT", [D, NTOK])
    yf_d = P.dram_in("yfT", [D, NTOK])
    yb_d = P.dram_in("ybT", [D, NTOK])
    nw_d = P.dram_in("nwT", [128, 16])
    sc_d = P.dram_in("scT", [128, 16, 2])
    sh_d = P.dram_in("shT", [128, 16, 2])
    g_d = P.dram_in("gT", [128, 16, 2])
    dsk_d = P.dram_in("dskT", [128, 16])
    wg_d = P.dram_in("wglu", [D, 2 * D])
    bg_d = P.dram_in("bgluT", [128, 32])
    o_d = P.dram_out("xoT", [D, NTOK])

    wg = P.sb("wg", [128, 16, 2 * D], BF16)
    xt = P.sb("xt", [128, 16, 128])
    sq = P.sb("sq", [128, 16, 128])
    yf = P.sb("yf", [128, 16, 128])
    yb = P.sb("yb", [128, 16, 128])
    gT = P.sb("gT_", [128, 16, 128], BF16)
    sg = [P.sb("sg%d" % i, [128, 128]) for i in range(2)]
    ot = [P.sb("ot%d" % i, [128, 128]) for i in range(2)]
    rsd = P.sb("rsd", [128, 128])
    ones32 = P.sb("ones32", [128, 128])
    nw = P.sb("nwT", [128, 16])
    A = P.sb("A", [128, 16, 2])
    B = P.sb("B", [128, 16, 2])
    G = P.sb("G", [128, 16, 2])
    dsk = P.sb("dsk", [128, 16])
    bg = P.sb("bg", [128, 32])
    banks = [P.bank() for _ in range(8)]
    bn = ["bank%d" % i for i in range(8)]
    for dst, src, nm in ((nw, nw_d, "nw"), (A, sc_d, "A"), (B, sh_d, "B"), (G, g_d, "G"), (dsk, dsk_d, "dsk"),
                         (bg, bg_d, "bg")):
        P.dma("sp", dst[:], src, writes=[nm])
    P.op("dve", lambda e: e.memset(ones32[:], 1.0), writes=["ones32"])
    for s in range(2):
        P.op("dve", lambda e, s=s: e.scalar_tensor_tensor(out=A[:, :, s], in0=A[:, :, s], scalar=1.0, in1=nw[:],
                                                          op0=ALU.add, op1=ALU.mult), reads=["A", "nw"], writes=["A"])
    for cb in range(8):
        P.dma("pool", wg[:, :, cb * 512:(cb + 1) * 512],
              wg_d[:, cb * 512:(cb + 1) * 512].rearrange("(k p) c -> p k c", p=128), writes=["wg"])

    def flat(t):
        return t[:].rearrange("p a b -> p (a b)")
    for t in range(18):
        seg = 0 if t < 16 else 1
        c0 = t * 128
        P.dma("sp", xt[:], xT_d[:, c0:c0 + 128].rearrange("(k p) c -> p k c", p=128), writes=["xt"])
        P.dma("sp", yf[:], yf_d[:, c0:c0 + 128].rearrange("(k p) c -> p k c", p=128), writes=["yf"])
        P.dma("sp", yb[:], yb_d[:, c0:c0 + 128].rearrange("(k p) c -> p k c", p=128), writes=["yb"])
        P.op("act", lambda e: e.activation(out=flat(sq), in_=flat(xt), func=AF.Square), reads=["xt"], writes=["sq"])

        def mm(e):
            ins = None
            for k in range(16):
                ins = e.matmul(banks[7][:, 0:128], lhsT=ones32[:], rhs=sq[:, k, :], start=(k == 0), stop=(k == 15))
            return ins
        P.op("pe", mm, reads=["sq", "ones32"], writes=[bn[7]])
        P.op("act", lambda e: e.activation(out=rsd[:], in_=banks[7][:, 0:128], func=AF.Sqrt, scale=1.0 / D, bias=EPS),
             reads=[bn[7]], writes=["rsd"])
        P.op("dve", lambda e: e.reciprocal(out=rsd[:], in_=rsd[:]), reads=["rsd"], writes=["rsd"])
        for k in range(16):
            P.op("dve", lambda e, k=k: e.scalar_tensor_tensor(out=sq[:, k, :], in0=xt[:, k, :],
                                                              scalar=A[:, k, seg:seg + 1], in1=rsd[:],
                                                              op0=ALU.mult, op1=ALU.mult),
                 reads=["xt", "A", "rsd"], writes=["sq"])
            P.op("dve", lambda e, k=k: e.tensor_scalar(out=sq[:, k, :], in0=sq[:, k, :], scalar1=B[:, k, seg:seg + 1],
                                                       scalar2=dsk[:, k:k + 1], op0=ALU.add, op1=ALU.mult),
                 reads=["sq", "B", "dsk"], writes=["sq"])
        P.op("dve", lambda e: e.tensor_tensor(out=flat(sq), in0=flat(sq), in1=flat(yf), op=ALU.add),
             reads=["sq", "yf"], writes=["sq"])
        P.op("dve", lambda e: e.tensor_tensor(out=flat(sq), in0=flat(sq), in1=flat(yb), op=ALU.add),
             reads=["sq", "yb"], writes=["sq"])
        P.op("act", lambda e: e.activation(out=flat(yf), in_=flat(sq), func=AF.Square), reads=["sq"], writes=["yf"])
        P.op("dve", lambda e: e.tensor_scalar(out=flat(yf), in0=flat(yf), scalar1=0.044715, scalar2=1.0,
                                              op0=ALU.mult, op1=ALU.add), reads=["yf"], writes=["yf"])
        P.op("dve", lambda e: e.tensor_tensor(out=flat(yf), in0=flat(yf), in1=flat(sq), op=ALU.mult),
             reads=["yf", "sq"], writes=["yf"])
        P.op("act", lambda e: e.activation(out=flat(yf), in_=flat(yf), func=AF.Sigmoid, scale=1.5957691216057308),
             reads=["yf"], writes=["yf"])
        P.op("dve", lambda e: e.tensor_tensor(out=flat(gT), in0=flat(yf), in1=flat(sq), op=ALU.mult),
             reads=["yf", "sq"], writes=["gT"])
        for dc in range(16):
            i2 = dc % 2
            b1, b2 = banks[2 * i2], banks[2 * i2 + 1]

            def zmm(e, b, c):
                ins = None
                for k in range(16):
                    ins = e.matmul(b[:, 0:128], lhsT=wg[:, k, c:c + 128], rhs=gT[:, k, :], start=(k == 0), stop=(k == 15))
                return ins
            P.op("pe", lambda e: zmm(e, b1, dc * 128), reads=["wg", "gT"], writes=[bn[2 * i2]])
            P.op("pe", lambda e: zmm(e, b2, D + dc * 128), reads=["wg", "gT"], writes=[bn[2 * i2 + 1]])
            P.op("act", lambda e: e.activation(out=sg[i2][:], in_=b2[:, 0:128], func=AF.Sigmoid,
                                               bias=bg[:, 16 + dc:17 + dc]),
                 reads=[bn[2 * i2 + 1], "bg"], writes=["sg%d" % i2])
            P.op("dve", lambda e: e.scalar_tensor_tensor(out=ot[i2][:], in0=b1[:, 0:128], scalar=bg[:, dc:dc + 1],
                                                         in1=sg[i2][:], op0=ALU.add, op1=ALU.mult),
                 reads=[bn[2 * i2], "bg", "sg%d" % i2], writes=["ot%d" % i2])
            P.op("dve", lambda e: e.scalar_tensor_tensor(out=yb[:, dc, :], in0=ot[i2][:], scalar=G[:, dc, seg:seg + 1],
                                                         in1=xt[:, dc, :], op0=ALU.mult, op1=ALU.add),
                 reads=["ot%d" % i2, "G", "xt"], writes=["yb"])
        P.dma("sp", o_d[:, c0:c0 + 128].rearrange("(k p) c -> p k c", p=128), yb[:], reads=["yb"])
    return P.finish()


_PROGS = {}


def _prog(name, fn):
    if name not in _PROGS:
        _PROGS[name] = fn()
    return _PROGS[name]


def _run(nc, maps):
    return run_bass_kernel_spmd(nc, maps, core_ids=list(range(8))).results


def kernel(x, c, ctx, c_ctx, w_mod, b_mod, norm_mix, norm_ffn, w_router, b_router, w_gate_up, b_gate_up,
           w_down, b_down, attn_w_qkv, attn_w_o, attn_q_gain, attn_k_gain, attn_sinks, ssm_a_re, ssm_a_im,
           ssm_log_dt, ssm_b_re, ssm_b_im, ssm_c_re, ssm_c_im, ssm_d, ssm_w_glu, ssm_b_glu):
    f = lambda a: np.asarray(a, np.float32)
    x = f(x).copy()
    ctx = f(ctx).copy()
    mod = run_mod(f(c), f(c_ctx), f(w_mod), f(b_mod)).reshape(4, 5, 6, D)
    SH_M, SC_M, G_M, SH_F, SC_F, G_F = range(6)
    for i in range(4):
        j = i // 2
        m = mod[i]
        if i % 2 == 0:
            nc = _prog("attn", build_attn)
            maps = []
            for core in range(8):
                b, hf = core // 2, core % 2
                maps.append(attn_inputs(x[b], ctx[b], hf, f(norm_mix[i]), m[b, SH_M], m[b, SC_M], m[b, G_M],
                                        m[4, SH_M], m[4, SC_M], m[4, G_M], f(attn_w_qkv[j]), f(attn_w_o[j]),
                                        f(attn_q_gain[j]), f(attn_k_gain[j]), f(attn_sinks[j])))
            res = _run(nc, maps)
            for core in range(8):
                b, hf = core // 2, core % 2
                xo = res[core]["xoT"]
                x[b, hf * 2048:(hf + 1) * 2048] = xo[:, :2048].T
                if hf == 0:
                    ctx[b] = xo[:, 2048:].T
        else:
            nc = _prog("s5", build_s5)
            maps = []
            for core in range(8):
                b, d = core // 2, core % 2
                seq = np.concatenate([ctx[b], x[b]], 0) if d == 0 else np.concatenate([ctx[b][::-1], x[b][::-1]], 0)
                maps.append(s5_inputs(np.ascontiguousarray(seq.T), f(norm_mix[i]), m[b, SH_M], m[b, SC_M],
                                      m[4, SH_M], m[4, SC_M], f(ssm_a_re[j, d]), f(ssm_a_im[j, d]),
                                      f(ssm_log_dt[j, d]), f(ssm_b_re[j, d]), f(ssm_b_im[j, d]),
                                      f(ssm_c_re[j, d]), f(ssm_c_im[j, d])))
            res = _run(nc, maps)
            ydir = {}
            for core in range(8):
                b, d = core // 2, core % 2
                y = res[core]["yT"].T
                yc, yl = y[:256], y[256:]
                if d == 1:
                    yc, yl = yc[::-1], yl[::-1]
                ydir[(b, d)] = (yc, yl)
            nc = _prog("glu", build_glu)
            maps = []
            for core in range(8):
                b, hf = core // 2, core % 2
                sl = slice(hf * 2048, (hf + 1) * 2048)
                cat = lambda lat, cx: np.ascontiguousarray(np.concatenate([lat, cx], 0).T)
                maps.append({
                    "xT": cat(x[b, sl], ctx[b]),
                    "yfT": cat(ydir[(b, 0)][1][sl], ydir[(b, 0)][0]),
                    "ybT": cat(ydir[(b, 1)][1][sl], ydir[(b, 1)][0]),
                    "nwT": fm(norm_mix[i]), "scT": fm2(m[b, SC_M], m[4, SC_M]), "shT": fm2(m[b, SH_M], m[4, SH_M]),
                    "gT": fm2(m[b, G_M], m[4, G_M]), "dskT": fm(ssm_d[j]), "wglu": f(ssm_w_glu[j]),
                    "bgluT": np.ascontiguousarray(f(ssm_b_glu[j]).reshape(32, 128).T),
                })
            res = _run(nc, maps)
            for core in range(8):
                b, hf = core // 2, core % 2
                xo = res[core]["xoT"]
                x[b, hf * 2048:(hf + 1) * 2048] = xo[:, :2048].T
                if hf == 0:
                    ctx[b] = xo[:, 2048:].T
        nc = _prog("moe", build_moe)
        maps = []
        for core in range(8):
            b, hf = core // 2, core % 2
            xt = np.concatenate([x[b, hf * 2048:(hf + 1) * 2048], ctx[b, hf * 128:(hf + 1) * 128]], 0)
            maps.append(moe_inputs(xt, f(norm_ffn[i]), np.stack([m[b, SC_F], m[4, SC_F]]),
                                   np.stack([m[b, SH_F], m[4, SH_F]]), np.stack([m[b, G_F], m[4, G_F]]),
                                   f(w_router[i]), f(b_router[i]), f(w_gate_up[i]), f(b_gate_up[i]),
                                   f(w_down[i]), f(b_down[i])))
        res = _run(nc, maps)
        for core in range(8):
            b, hf = core // 2, core % 2
            y = res[core]["y"]
            x[b, hf * 2048:(hf + 1) * 2048] = y[:2048]
            ctx[b, hf * 128:(hf + 1) * 128] = y[2048:]
    return x
```

```python
import numpy as np
import concourse.bass as bass
import concourse.mybir as mybir
from concourse.bass_utils import run_bass_kernel_spmd

F32, BF16 = mybir.dt.float32, mybir.dt.bfloat16
AF = mybir.ActivationFunctionType
ALU = mybir.AluOpType

D = 2048
NE = 32
FE = 1024
EPS = 1e-6


class Prog:
    NDS = 24

    def __init__(self):
        self.nc = bass.Bass("TRN2", target_bir_lowering=False)
        nc = self.nc
        self.E = {"pe": nc.tensor, "act": nc.scalar, "dve": nc.vector, "pool": nc.gpsimd, "sp": nc.sync}
        self.sem = {e: nc.semaphore("sem_" + e).__enter__() for e in ("pe", "act", "dve", "pool")}
        self.cnt = {e: 0 for e in self.sem}
        self.waited = {}
        self.lastw = {}
        self.readers = {}
        self.dsems = [nc.semaphore("dsem%d" % i).__enter__() for i in range(self.NDS)]
        self.dcount = [0] * self.NDS
        self.dnext = 0
        self.nbank = 0

    def sb(self, name, shape, dt=F32):
        return self.nc.sbuf_tensor("s_" + name, list(shape), dt).__enter__()

    def bank(self, name=None):
        self.nbank += 1
        return self.nc.psum_tensor(name or ("bank%d" % self.nbank), [128, 512], F32).__enter__()

    def dram_in(self, name, shape, dt=F32):
        return self.nc.dram_tensor(name, list(shape), dt, kind="ExternalInput").ap()

    def dram_out(self, name, shape, dt=F32):
        return self.nc.dram_tensor(name, list(shape), dt, kind="ExternalOutput").ap()

    def _deps(self, reads, writes):
        toks = []
        for r in reads:
            if r in self.lastw:
                toks.append(self.lastw[r])
        for w in writes:
            if w in self.lastw:
                toks.append(self.lastw[w])
            toks.extend(self.readers.get(w, {}).values())
        return toks

    def _wait(self, e, toks):
        need = {}
        for s, v in toks:
            k = id(s)
            if k not in need or need[k][1] < v:
                need[k] = (s, v)
        for k, (s, v) in need.items():
            if self.waited.get((e, k), 0) < v:
                self.E[e].wait_ge(s, v)
                self.waited[(e, k)] = v

    def _commit(self, tok, reads, writes):
        for w in writes:
            self.lastw[w] = tok
            self.readers[w] = {}
        for r in reads:
            d = self.readers.setdefault(r, {})
            k = id(tok[0])
            if k not in d or d[k][1] < tok[1]:
                d[k] = tok

    def op(self, e, fn, reads=(), writes=()):
        toks = self._deps(reads, writes)
        if e == "pe":
            toks = [t for t in toks if t[0] is not self.sem["pe"]]
        self._wait(e, toks)
        inst = fn(self.E[e])
        self.cnt[e] += 1
        inst.then_inc(self.sem[e], 1)
        self._commit((self.sem[e], self.cnt[e]), reads, writes)

    def dma(self, q, out, in_, reads=(), writes=(), **kw):
        toks = self._deps(reads, writes)
        slot = self.dnext
        self.dnext = (slot + 1) % self.NDS
        if self.dcount[slot] > 0:
            toks.append((self.dsems[slot], 16 * self.dcount[slot]))
        self._wait(q, toks)
        self.E[q].dma_start(out=out, in_=in_, **kw).then_inc(self.dsems[slot], 16)
        self.dcount[slot] += 1
        self._commit((self.dsems[slot], 16 * self.dcount[slot]), reads, writes)

    def finish(self):
        toks = [(self.dsems[i], 16 * self.dcount[i]) for i in range(self.NDS) if self.dcount[i] > 0]
        self._wait("sp", toks)
        return self.nc


def build_moe(NT=17, n_lat=16, ne=NE, passes=None):
    P = Prog()
    nc = P.nc
    if passes is None:
        passes = [list(range(0, 5)), list(range(5, 9)), list(range(9, 13)), list(range(13, 17))]
    x_d = P.dram_in("x", [NT * 128, D])
    nw_d = P.dram_in("nw_b", [128, D])
    sc_d = P.dram_in("sc_b", [2, 128, D])
    sh_d = P.dram_in("sh_b", [2, 128, D])
    gf_d = P.dram_in("gf_b", [2, 128, D])
    wr_d = P.dram_in("wr", [128, 16, ne])
    br_d = P.dram_in("br_b", [128, ne])
    wgu_d = P.dram_in("wgu", [ne, D, 2 * FE])
    bgu_d = P.dram_in("bgu", [128, ne, 2, 8])
    wdn_d = P.dram_in("wdn", [ne, FE, D])
    bdn_d = P.dram_in("bdn", [ne, D])
    id_d = P.dram_in("ident", [128, 128])
    y_d = P.dram_out("y", [NT * 128, D])

    maxt = max(len(p) for p in passes)
    ident = P.sb("ident", [128, 128])
    A_b = P.sb("A_b", [128, D])
    B_b = P.sb("B_b", [128, D])
    G_b = [P.sb("G_b%d" % s, [128, D]) for s in range(2)]
    wr = P.sb("wr_s", [128, 16, ne])
    br_b = P.sb("br_s", [128, ne])
    bgu = P.sb("bgu_s", [128, ne, 2, 8])
    bdn = P.sb("bdn_s", [ne, D])
    hT = P.sb("hT", [128, 16, maxt * 128], BF16)
    actTs = [P.sb("actT%d" % i, [128, 8, maxt * 128], BF16) for i in range(2)]
    acc = P.sb("acc", [128, maxt, D])
    gates = P.sb("gates", [128, maxt, ne])
    gT = P.sb("gT", [ne, maxt * 128])
    xt = P.sb("xt", [128, D])
    h32T = P.sb("h32T", [128, 16, 128])
    lg = P.sb("lg", [128, ne])
    m8 = P.sb("m8", [128, 8])
    sm = P.sb("sm", [128, 8])
    NGU, NDN = 3, 3
    wg = [P.sb("wg%d" % i, [128, 16, 256], BF16) for i in range(NGU)]
    wd = [P.sb("wd%d" % i, [128, 8, 512], BF16) for i in range(NDN)]
    tg = [P.sb("tg%d" % i, [128, 512]) for i in range(2)]
    ts_ = [P.sb("ts%d" % i, [128, 512]) for i in range(2)]
    tu = [P.sb("tu%d" % i, [128, 512]) for i in range(2)]
    banks = [P.bank() for _ in range(8)]

    P.dma("sp", ident[:], id_d, writes=["ident"])
    P.dma("sp", wr[:], wr_d, writes=["wr"])
    P.dma("sp", br_b[:], br_d, writes=["br"])
    P.dma("sp", bgu[:], bgu_d, writes=["bgu"])
    P.dma("sp", bdn[:], bdn_d, writes=["bdn"])
    for s in range(2):
        P.dma("sp", G_b[s][:], gf_d[s], writes=["G%d" % s])

    cur_seg = [-1]

    def load_seg(s):
        if cur_seg[0] == s:
            return
        cur_seg[0] = s
        P.dma("sp", A_b[:], sc_d[s], writes=["A"])
        P.dma("sp", B_b[:], nw_d, writes=["B"])
        P.op("dve", lambda e: e.scalar_tensor_tensor(out=A_b[:], in0=A_b[:], scalar=1.0, in1=B_b[:],
                                                     op0=ALU.add, op1=ALU.mult),
             reads=["A", "B"], writes=["A"])
        P.dma("sp", B_b[:], sh_d[s], writes=["B"])

    wq = {"gu": 0, "dn": 0}

    def load_gu(e_, fc):
        i = wq["gu"] % NGU
        wq["gu"] += 1
        src = wgu_d[e_, :, fc * 256:(fc + 1) * 256].rearrange("(k p) c -> p k c", p=128)
        P.dma("pool", wg[i][:], src, writes=["wg%d" % i])
        return i

    def load_dn(e_, db):
        i = wq["dn"] % NDN
        wq["dn"] += 1
        src = wdn_d[e_, :, db * 512:(db + 1) * 512].rearrange("(k p) c -> p k c", p=128)
        P.dma("pool", wd[i][:], src, writes=["wd%d" % i])
        return i

    bk = {"gu": 0, "dn": 0, "misc": 0}

    for tiles in passes:
        npt = len(tiles)
        for j, t in enumerate(tiles):
            load_seg(0 if t < n_lat else 1)
            P.dma("sp", xt[:], x_d[t * 128:(t + 1) * 128, :], writes=["xt"])
            P.op("act", lambda e: e.activation(out=h32T[:].rearrange("p a b -> p (a b)"), in_=xt[:], func=AF.Square,
                                               accum_out=sm[:, 0:1]),
                 reads=["xt"], writes=["h32T", "sm0"])
            P.op("dve", lambda e: e.tensor_scalar(out=sm[:, 1:2], in0=sm[:, 0:1], scalar1=1.0 / D, scalar2=EPS,
                                                  op0=ALU.mult, op1=ALU.add), reads=["sm0"], writes=["sm1"])
            P.op("act", lambda e: e.activation(out=sm[:, 2:3], in_=sm[:, 1:2], func=AF.Sqrt),
                 reads=["sm1"], writes=["sm2"])
            P.op("dve", lambda e: e.reciprocal(out=sm[:, 3:4], in_=sm[:, 2:3]), reads=["sm2"], writes=["sm3"])
            P.op("dve", lambda e: e.scalar_tensor_tensor(out=xt[:], in0=xt[:], scalar=sm[:, 3:4], in1=A_b[:],
                                                         op0=ALU.mult, op1=ALU.mult),
                 reads=["xt", "sm3", "A"], writes=["xt"])
            P.op("dve", lambda e: e.tensor_tensor(out=xt[:], in0=xt[:], in1=B_b[:], op=ALU.add),
                 reads=["xt", "B"], writes=["xt"])
            for q in range(4):
                b = banks[4 + (bk["misc"] % 2)]
                bn = "bank%d" % (4 + (bk["misc"] % 2))
                bk["misc"] += 1

                def tr(e, q=q, b=b):
                    ins = None
                    for r in range(4):
                        k = q * 4 + r
                        ins = e.transpose(b[:, r * 128:(r + 1) * 128], xt[:, k * 128:(k + 1) * 128], ident[:])
                    return ins
                P.op("pe", tr, reads=["xt", "ident"], writes=[bn])
                P.op("act", lambda e, q=q, b=b: e.activation(
                    out=h32T[:, q * 4:(q + 1) * 4, :].rearrange("p a b -> p (a b)"), in_=b[:], func=AF.Identity),
                    reads=[bn], writes=["h32T"])
            P.op("pool", lambda e, j=j: e.tensor_copy(out=hT[:, :, j * 128:(j + 1) * 128], in_=h32T[:]),
                 reads=["h32T"], writes=["hT"])
            b = banks[6]

            def rt(e, b=b):
                ins = None
                for k in range(16):
                    ins = e.matmul(b[:, 0:ne], lhsT=h32T[:, k, :], rhs=wr[:, k, :], start=(k == 0), stop=(k == 15))
                return ins
            P.op("pe", rt, reads=["h32T", "wr"], writes=["bank6"])
            P.op("dve", lambda e, b=b: e.tensor_tensor(out=lg[:], in0=b[:, 0:ne], in1=br_b[:], op=ALU.add),
                 reads=["bank6", "br"], writes=["lg"])
            P.op("dve", lambda e: e.max(out=m8[:], in_=lg[:]), reads=["lg"], writes=["m8"])
            P.op("dve", lambda e: e.tensor_scalar(out=sm[:, 4:5], in0=m8[:, 0:1], scalar1=-1.0, scalar2=None,
                                                  op0=ALU.mult), reads=["m8"], writes=["sm4"])
            P.op("act", lambda e, j=j: e.activation(out=gates[:, j, :], in_=lg[:], func=AF.Exp, bias=sm[:, 4:5]),
                 reads=["lg", "sm4"], writes=["gates"])
            P.op("dve", lambda e: e.tensor_scalar(out=lg[:], in0=lg[:], scalar1=m8[:, 3:4], scalar2=None,
                                                  op0=ALU.is_ge), reads=["lg", "m8"], writes=["lg"])
            P.op("dve", lambda e, j=j: e.tensor_tensor(out=gates[:, j, :], in0=gates[:, j, :], in1=lg[:],
                                                       op=ALU.mult), reads=["gates", "lg"], writes=["gates"])
            P.op("dve", lambda e, j=j: e.reduce_sum(out=sm[:, 5:6], in_=gates[:, j, :],
                                                    axis=mybir.AxisListType.X), reads=["gates"], writes=["sm5"])
            P.op("dve", lambda e: e.reciprocal(out=sm[:, 6:7], in_=sm[:, 5:6]), reads=["sm5"], writes=["sm6"])
            P.op("dve", lambda e, j=j: e.tensor_scalar(out=gates[:, j, :], in0=gates[:, j, :], scalar1=sm[:, 6:7],
                                                       scalar2=None, op0=ALU.mult),
                 reads=["gates", "sm6"], writes=["gates"])
            b = banks[7]
            P.op("pe", lambda e, j=j, b=b: e.transpose(b[0:ne, 0:128], gates[:, j, :], ident[:]),
                 reads=["gates", "ident"], writes=["bank7"])
            P.op("act", lambda e, j=j, b=b: e.activation(out=gT[:, j * 128:(j + 1) * 128], in_=b[0:ne, 0:128],
                                                         func=AF.Identity), reads=["bank7"], writes=["gT"])
        for j in range(npt):
            for db in range(4):
                b = banks[4 + (bk["misc"] % 2)]
                bn = "bank%d" % (4 + (bk["misc"] % 2))
                bk["misc"] += 1
                P.op("pe", lambda e, j=j, db=db, b=b: e.matmul(b[:], lhsT=gT[:, j * 128:(j + 1) * 128],
                                                               rhs=bdn[:, db * 512:(db + 1) * 512],
                                                               start=True, stop=True),
                     reads=["gT", "bdn"], writes=[bn])
                P.op("act", lambda e, j=j, db=db, b=b: e.activation(out=acc[:, j, db * 512:(db + 1) * 512],
                                                                    in_=b[:], func=AF.Identity),
                     reads=[bn], writes=["acc%d" % j])
        if npt * 128 <= 512:
            blocks = [(0, npt * 128)]
        else:
            h1 = ((npt + 1) // 2) * 128
            blocks = [(0, h1), (h1, npt * 128)]
        units = []
        for e_ in range(ne + 1):
            if e_ < ne:
                units += [("gu", e_, fc) for fc in range(8)]
            if e_ >= 1:
                units += [("dn", e_ - 1, db) for db in range(4)]
        loaded = {}

        def ensure(i):
            if i < len(units) and i not in loaded:
                kind, ee, ii = units[i]
                loaded[i] = load_gu(ee, ii) if kind == "gu" else load_dn(ee, ii)
        for ui, (kind, e_, idx) in enumerate(units):
            ensure(ui)
            ensure(ui + 1)
            ensure(ui + 2)
            wi = loaded.pop(ui)
            actT = actTs[e_ % 2]
            an = "actT%d_" % (e_ % 2)
            if kind == "gu":
                fc = idx
                for (c0, c1) in blocks:
                    n = c1 - c0
                    i2 = bk["gu"] % 2
                    bk["gu"] += 1
                    bg_, bu_ = banks[2 * i2], banks[2 * i2 + 1]
                    bgn, bun = "bank%d" % (2 * i2), "bank%d" % (2 * i2 + 1)

                    def mm(e, par, b):
                        ins = None
                        for k in range(16):
                            ins = e.matmul(b[:, 0:n], lhsT=wg[wi][:, k, par:256:2], rhs=hT[:, k, c0:c1],
                                           start=(k == 0), stop=(k == 15))
                        return ins
                    P.op("pe", lambda e: mm(e, 0, bg_), reads=["wg%d" % wi, "hT"], writes=[bgn])
                    P.op("pe", lambda e: mm(e, 1, bu_), reads=["wg%d" % wi, "hT"], writes=[bun])
                    g_, s_, u_ = tg[i2], ts_[i2], tu[i2]
                    gn, sn, un = "tg%d" % i2, "ts%d" % i2, "tu%d" % i2
                    P.op("dve", lambda e: e.tensor_scalar(out=g_[:, 0:n], in0=bg_[:, 0:n],
                                                          scalar1=bgu[:, e_, 0, fc:fc + 1], scalar2=7.0,
                                                          op0=ALU.add, op1=ALU.min),
                         reads=[bgn, "bgu"], writes=[gn])
                    P.op("act", lambda e: e.activation(out=s_[:, 0:n], in_=g_[:, 0:n], func=AF.Sigmoid, scale=1.702),
                         reads=[gn], writes=[sn])
                    P.op("dve", lambda e: e.tensor_scalar(out=u_[:, 0:n], in0=bu_[:, 0:n],
                                                          scalar1=bgu[:, e_, 1, fc:fc + 1], scalar2=-7.0,
                                                          op0=ALU.add, op1=ALU.max),
                         reads=[bun, "bgu"], writes=[un])
                    P.op("dve", lambda e: e.tensor_scalar(out=u_[:, 0:n], in0=u_[:, 0:n], scalar1=7.0, scalar2=1.0,
                                                          op0=ALU.min, op1=ALU.add), reads=[un], writes=[un])
                    P.op("dve", lambda e: e.tensor_tensor(out=g_[:, 0:n], in0=g_[:, 0:n], in1=s_[:, 0:n],
                                                          op=ALU.mult), reads=[gn, sn], writes=[gn])
                    P.op("dve", lambda e: e.tensor_tensor(out=actT[:, fc, c0:c1], in0=g_[:, 0:n], in1=u_[:, 0:n],
                                                          op=ALU.mult), reads=[gn, un], writes=[an + str(fc)])
            else:
                db = idx
                for j in range(npt):
                    i3 = 4 + (bk["dn"] % 3)
                    bk["dn"] += 1
                    b = banks[i3]
                    bn = "bank%d" % i3

                    def dn(e):
                        ins = None
                        for fc2 in range(8):
                            ins = e.matmul(b[:], lhsT=actT[:, fc2, j * 128:(j + 1) * 128], rhs=wd[wi][:, fc2, :],
                                           start=(fc2 == 0), stop=(fc2 == 7))
                        return ins
                    P.op("pe", dn, reads=["wd%d" % wi] + [an + str(f) for f in range(8)], writes=[bn])
                    P.op("dve", lambda e: e.scalar_tensor_tensor(
                        out=acc[:, j, db * 512:(db + 1) * 512], in0=b[:], scalar=gates[:, j, e_:e_ + 1],
                        in1=acc[:, j, db * 512:(db + 1) * 512], op0=ALU.mult, op1=ALU.add),
                        reads=[bn, "gates", "acc%d" % j], writes=["acc%d" % j])
        for j, t in enumerate(tiles):
            s = 0 if t < n_lat else 1
            P.dma("sp", xt[:], x_d[t * 128:(t + 1) * 128, :], writes=["xt"])
            P.op("pool", lambda e, j=j, s=s: e.tensor_tensor(out=acc[:, j, :], in0=acc[:, j, :], in1=G_b[s][:],
                                                             op=ALU.mult), reads=["acc%d" % j, "G%d" % s],
                 writes=["acc%d" % j])
            P.op("pool", lambda e, j=j: e.tensor_tensor(out=acc[:, j, :], in0=acc[:, j, :], in1=xt[:], op=ALU.add),
                 reads=["acc%d" % j, "xt"], writes=["acc%d" % j])
            P.dma("sp", y_d[t * 128:(t + 1) * 128, :], acc[:, j, :], reads=["acc%d" % j])
    return P.finish()


def bcast(v):
    return np.ascontiguousarray(np.broadcast_to(np.asarray(v, np.float32)[None, :], (128, v.shape[-1])))


def moe_inputs(x_tiles, nw, sc, sh, gf, w_router, b_router, w_gu, b_gu, w_dn, b_dn):
    ne = w_router.shape[1]
    return {
        "x": np.ascontiguousarray(x_tiles, np.float32),
        "nw_b": bcast(nw),
        "sc_b": np.stack([bcast(sc[0]), bcast(sc[1])]),
        "sh_b": np.stack([bcast(sh[0]), bcast(sh[1])]),
        "gf_b": np.stack([bcast(gf[0]), bcast(gf[1])]),
        "wr": np.ascontiguousarray(w_router.reshape(16, 128, ne).transpose(1, 0, 2)),
        "br_b": bcast(b_router),
        "wgu": w_gu,
        "bgu": np.ascontiguousarray(b_gu.reshape(ne, 8, 128, 2).transpose(2, 0, 3, 1)),
        "wdn": w_dn,
        "bdn": b_dn,
        "ident": np.eye(128, dtype=np.float32),
    }


def build_mod():
    P = Prog()
    wm_d = P.dram_in("wm", [D, 6144])
    cT_d = P.dram_in("cT", [128, 16, 5])
    bm_d = P.dram_in("bmT", [128, 48])
    o_d = P.dram_out("modT", [128, 48, 5])
    cs = P.sb("cs", [128, 16, 5])
    bm = P.sb("bm", [128, 48])
    res = P.sb("res", [128, 48, 5])
    st = [P.sb("st%d" % i, [128, 16, 256]) for i in range(3)]
    banks = [P.bank() for _ in range(2)]
    P.dma("sp", cs[:], cT_d, writes=["cs"])
    P.dma("sp", bm[:], bm_d, writes=["bm"])
    P.op("act", lambda e: e.activation(out=cs[:].rearrange("p a b -> p (a b)"),
                                       in_=cs[:].rearrange("p a b -> p (a b)"), func=AF.Silu),
         reads=["cs"], writes=["cs"])
    for cb in range(24):
        s = st[cb % 3]
        sn = "st%d" % (cb % 3)
        P.dma("sp", s[:], wm_d[:, cb * 256:(cb + 1) * 256].rearrange("(k p) c -> p k c", p=128), writes=[sn])
        for dc in range(2):
            ch = cb * 2 + dc
            b = banks[ch % 2]
            bn = "bank%d" % (ch % 2)

            def mm(e, b=b, s=s, dc=dc):
                ins = None
                for k in range(16):
                    ins = e.matmul(b[:, 0:5], lhsT=s[:, k, dc * 128:(dc + 1) * 128], rhs=cs[:, k, :],
                                   start=(k == 0), stop=(k == 15))
                return ins
            P.op("pe", mm, reads=[sn, "cs"], writes=[bn])
            P.op("dve", lambda e, b=b, ch=ch: e.tensor_scalar(out=res[:, ch, :], in0=b[:, 0:5],
                                                              scalar1=bm[:, ch:ch + 1], scalar2=None, op0=ALU.add),
                 reads=[bn, "bm"], writes=["res"])
    P.dma("sp", o_d, res[:], reads=["res"])
    return P.finish()


def run_mod(c, c_ctx, w_mod, b_mod):
    nc = build_mod()
    rows = np.concatenate([c, c_ctx[None, :]], 0).astype(np.float32)
    cT = np.ascontiguousarray(rows.T.reshape(16, 128, 5).transpose(1, 0, 2))
    maps = []
    for core in range(8):
        l, hf = core // 2, core % 2
        maps.append({
            "wm": np.ascontiguousarray(w_mod[l][:, hf * 6144:(hf + 1) * 6144]),
            "cT": cT,
            "bmT": np.ascontiguousarray(b_mod[l][hf * 6144:(hf + 1) * 6144].reshape(48, 128).T),
        })
    res = run_bass_kernel_spmd(nc, maps, core_ids=list(range(8)))
    mod = np.zeros((4, 5, 12288), np.float32)
    for core in range(8):
        l, hf = core // 2, core % 2
        m = res.results[core]["modT"]
        mod[l, :, hf * 6144:(hf + 1) * 6144] = m.transpose(2, 1, 0).reshape(5, 6144)
    return mod


HD = 128
ATT_NT = 20


def build_attn():
    P = Prog()
    NTOK = ATT_NT * 128
    xT_d = P.dram_in("xT", [D, NTOK])
    nw_d = P.dram_in("nwT", [128, 16])
    sc_d = P.dram_in("scT", [128, 16, 2])
    sh_d = P.dram_in("shT", [128, 16, 2])
    g_d = P.dram_in("gT", [128, 16, 2])
    wqkv_d = P.dram_in("wqkv", [D, 3072])
    wo_d = P.dram_in("wo", [D, D])
    qg_d = P.dram_in("qg", [128, 1])
    kg_d = P.dram_in("kg", [128, 1])
    sink_d = P.dram_in("sinkrep", [1, 4, 512])
    cos_d = P.dram_in("cosrep", [18, 128, 512])
    sin_d = P.dram_in("sinrep", [18, 128, 512])
    rm_d = P.dram_in("rmat", [128, 128])
    mask_d = P.dram_in("masks", [4, 128, 512])
    id_d = P.dram_in("ident", [128, 128])
    o_d = P.dram_out("xoT", [D, 18 * 128])

    wq_s = P.sb("wq", [128, 16, 1024], BF16)
    wo_s = P.sb("wo", [128, 16, 2048], BF16)
    kT = P.sb("kT", [128, 4, NTOK], BF16)
    v_s = P.sb("v", [128, ATT_NT, 512], BF16)
    xt = P.sb("xt", [128, 16, 128])
    sq = P.sb("sq", [128, 16, 128])
    uT = P.sb("uT", [128, 16, 128], BF16)
    qT = P.sb("qT", [128, 16, 128], BF16)
    oT = P.sb("oT", [128, 16, 128], BF16)
    pT = P.sb("pT", [128, 5, 512], BF16)
    t1 = P.sb("t1", [128, 512])
    t2 = P.sb("t2", [128, 512])
    t3 = P.sb("t3", [128, 512])
    ct = P.sb("ct", [128, 512])
    st = P.sb("st", [128, 512])
    rsd = P.sb("rsd", [128, 128])
    nw = P.sb("nwT", [128, 16])
    A = P.sb("A", [128, 16, 2])
    B = P.sb("B", [128, 16, 2])
    G = P.sb("G", [128, 16, 2])
    qg = P.sb("qg", [128, 1])
    kg = P.sb("kg", [128, 1])
    sink32 = P.sb("sink32", [1, 4, 512])
    sinkb = P.sb("sinkb", [1, 4, 512], BF16)
    rmat = P.sb("rmat", [128, 128])
    m32 = P.sb("m32", [128, 512])
    maskb = P.sb("maskb", [128, 4, 512], BF16)
    id32 = P.sb("id32", [128, 128])
    idb = P.sb("idb", [128, 128], BF16)
    ones32 = P.sb("ones32", [128, 128])
    onesb = P.sb("onesb", [128, 128], BF16)
    banks = [P.bank() for _ in range(8)]
    bn = ["bank%d" % i for i in range(8)]

    for dst, src, nm in ((nw, nw_d, "nw"), (A, sc_d, "A"), (B, sh_d, "B"), (G, g_d, "G"), (qg, qg_d, "qg"),
                         (kg, kg_d, "kg"), (sink32, sink_d, "sink32"), (rmat, rm_d, "rmat"), (id32, id_d, "id32")):
        P.dma("sp", dst[:], src, writes=[nm])
    P.op("dve", lambda e: e.tensor_copy(out=idb[:], in_=id32[:]), reads=["id32"], writes=["idb"])
    P.op("dve", lambda e: e.memset(ones32[:], 1.0), writes=["ones32"])
    P.op("dve", lambda e: e.memset(onesb[:], 1.0), writes=["onesb"])
    P.op("act", lambda e: e.activation(out=sinkb[:].rearrange("p a b -> p (a b)"),
                                       in_=sink32[:].rearrange("p a b -> p (a b)"), func=AF.Exp),
         reads=["sink32"], writes=["sinkb"])
    for i in range(4):
        P.dma("sp", m32[:], mask_d[i], writes=["m32"])
        P.op("dve", lambda e, i=i: e.tensor_copy(out=maskb[:, i, :], in_=m32[:]), reads=["m32"], writes=["maskb"])
    for s in range(2):
        P.op("dve", lambda e, s=s: e.scalar_tensor_tensor(out=A[:, :, s], in0=A[:, :, s], scalar=1.0, in1=nw[:],
                                                          op0=ALU.add, op1=ALU.mult),
             reads=["A", "nw"], writes=["A"])
    P.dma("pool", wq_s[:, :, 0:512], wqkv_d[:, 2048:2560].rearrange("(k p) c -> p k c", p=128), writes=["wq"])
    P.dma("pool", wq_s[:, :, 512:1024], wqkv_d[:, 2560:3072].rearrange("(k p) c -> p k c", p=128), writes=["wq"])
    for hh in range(4):
        P.dma("pool", wo_s[:, hh * 4:(hh + 1) * 4, :],
              wo_d[hh * 512:(hh + 1) * 512, :].rearrange("(h p) c -> p h c", p=128), writes=["wo"])

    def prenorm(t, seg):
        c0 = t * 128
        P.dma("sp", xt[:], xT_d[:, c0:c0 + 128].rearrange("(k p) c -> p k c", p=128), writes=["xt"])
        P.op("act", lambda e: e.activation(out=sq[:].rearrange("p a b -> p (a b)"),
                                           in_=xt[:].rearrange("p a b -> p (a b)"), func=AF.Square),
             reads=["xt"], writes=["sq"])

        def mm(e):
            ins = None
            for k in range(16):
                ins = e.matmul(banks[7][:, 0:128], lhsT=ones32[:], rhs=sq[:, k, :], start=(k == 0), stop=(k == 15))
            return ins
        P.op("pe", mm, reads=["sq", "ones32"], writes=[bn[7]])
        P.op("act", lambda e: e.activation(out=rsd[:], in_=banks[7][:, 0:128], func=AF.Sqrt, scale=1.0 / D, bias=EPS),
             reads=[bn[7]], writes=["rsd"])
        P.op("dve", lambda e: e.reciprocal(out=rsd[:], in_=rsd[:]), reads=["rsd"], writes=["rsd"])
        for k in range(16):
            P.op("dve", lambda e, k=k: e.scalar_tensor_tensor(out=sq[:, k, :], in0=xt[:, k, :],
                                                              scalar=A[:, k, seg:seg + 1], in1=rsd[:],
                                                              op0=ALU.mult, op1=ALU.mult),
                 reads=["xt", "A", "rsd"], writes=["sq"])
            P.op("act", lambda e, k=k: e.activation(out=uT[:, k, :], in_=sq[:, k, :], func=AF.Identity,
                                                    bias=B[:, k, seg:seg + 1]),
                 reads=["sq", "B"], writes=["uT"])

    def norm_rope(src_bank, src_name, gain, rope_tile, dst3, dst_name):
        P.op("act", lambda e: e.activation(out=t1[:], in_=src_bank[:], func=AF.Square), reads=[src_name], writes=["t1"])
        P.op("pe", lambda e: e.matmul(banks[7][:], lhsT=ones32[:], rhs=t1[:], start=True, stop=True),
             reads=["t1", "ones32"], writes=[bn[7]])
        P.op("act", lambda e: e.activation(out=t2[:], in_=banks[7][:], func=AF.Sqrt, scale=1.0 / HD, bias=EPS),
             reads=[bn[7]], writes=["t2"])
        P.op("dve", lambda e: e.reciprocal(out=t2[:], in_=t2[:]), reads=["t2"], writes=["t2"])
        dst2 = dst3
        if rope_tile is None:
            P.op("dve", lambda e: e.scalar_tensor_tensor(out=dst2, in0=src_bank[:].rearrange("p (a b) -> p a b", a=4),
                                                         scalar=gain[:, 0:1],
                                                         in1=t2[:].rearrange("p (a b) -> p a b", a=4),
                                                         op0=ALU.mult, op1=ALU.mult),
                 reads=[src_name, "t2", "qg", "kg"], writes=[dst_name])
            return
        P.op("dve", lambda e: e.scalar_tensor_tensor(out=t1[:], in0=src_bank[:], scalar=gain[:, 0:1], in1=t2[:],
                                                     op0=ALU.mult, op1=ALU.mult),
             reads=[src_name, "t2", "qg", "kg"], writes=["t1"])
        P.dma("sp", ct[:], cos_d[rope_tile], writes=["ct"])
        P.dma("sp", st[:], sin_d[rope_tile], writes=["st"])
        P.op("pe", lambda e: e.matmul(banks[7][:], lhsT=rmat[:], rhs=t1[:], start=True, stop=True),
             reads=["t1", "rmat"], writes=[bn[7]])
        P.op("dve", lambda e: e.tensor_tensor(out=t3[:], in0=banks[7][:], in1=st[:], op=ALU.mult),
             reads=[bn[7], "st"], writes=["t3"])
        P.op("dve", lambda e: e.tensor_tensor(out=t1[:], in0=t1[:], in1=ct[:], op=ALU.mult),
             reads=["t1", "ct"], writes=["t1"])
        P.op("dve", lambda e: e.tensor_tensor(out=dst2, in0=t1[:].rearrange("p (a b) -> p a b", a=4),
                                              in1=t3[:].rearrange("p (a b) -> p a b", a=4), op=ALU.add),
             reads=["t1", "t3"], writes=[dst_name])

    for t in range(ATT_NT):
        seg = 0 if t < 18 else 1
        prenorm(t, seg)

        def kmm(e):
            ins = None
            for g in range(4):
                for k in range(16):
                    ins = e.matmul(banks[0][:, g * 128:(g + 1) * 128], lhsT=wq_s[:, k, g * 128:(g + 1) * 128],
                                   rhs=uT[:, k, :], start=(k == 0), stop=(k == 15))
            return ins
        P.op("pe", kmm, reads=["wq", "uT"], writes=[bn[0]])

        def vmm(e):
            ins = None
            for k in range(16):
                ins = e.matmul(banks[1][:], lhsT=uT[:, k, :], rhs=wq_s[:, k, 512:1024], start=(k == 0), stop=(k == 15))
            return ins
        P.op("pe", vmm, reads=["wq", "uT"], writes=[bn[1]])
        P.op("act", lambda e, t=t: e.activation(out=v_s[:, t, :], in_=banks[1][:], func=AF.Identity),
             reads=[bn[1]], writes=["v"])
        norm_rope(banks[0], bn[0], kg, t if t < 18 else None, kT[:, :, t * 128:(t + 1) * 128], "kT")

    scale = HD ** -0.5
    for qi, t in enumerate(list(range(1, 17)) + [18, 19]):
        seg = 0 if t < 18 else 1
        prenorm(t, seg)
        for hh in range(2):
            P.dma("pool", wq_s[:], wqkv_d[:, hh * 1024:(hh + 1) * 1024].rearrange("(k p) c -> p k c", p=128),
                  writes=["wq"])
            for gg in range(2):
                b = banks[gg]

                def qmm(e, b=b, gg=gg):
                    ins = None
                    for h in range(4):
                        for k in range(16):
                            c = (gg * 4 + h) * 128
                            ins = e.matmul(b[:, h * 128:(h + 1) * 128], lhsT=wq_s[:, k, c:c + 128], rhs=uT[:, k, :],
                                           start=(k == 0), stop=(k == 15))
                    return ins
                P.op("pe", qmm, reads=["wq", "uT"], writes=[bn[gg]])
                g4 = (hh * 2 + gg) * 4
                norm_rope(b, bn[gg], qg, t if t < 18 else None, qT[:, g4:g4 + 4, :], "qT")
        if t < 18:
            kbs = [(t - 1, 2 if t == 1 else 0), (t, None), (t + 1, 3 if t == 16 else 1), (18, None), (19, None)]
        else:
            kbs = [(18, None), (19, None)]
        for g in range(4):
            q2 = qT[:, g * 4:(g + 1) * 4, :].rearrange("p a b -> p (a b)")
            for i, (kt, mk) in enumerate(kbs):
                sb_ = banks[2 + (i % 2)]
                sbn = bn[2 + (i % 2)]

                def smm(e, sb_=sb_, kt=kt, mk=mk):
                    ins = e.matmul(sb_[:], lhsT=kT[:, g, kt * 128:(kt + 1) * 128], rhs=q2, start=True, stop=(mk is None))
                    if mk is not None:
                        ins = e.matmul(sb_[:], lhsT=idb[:], rhs=maskb[:, mk, :], start=False, stop=True)
                    return ins
                P.op("pe", smm, reads=["kT", "qT", "idb", "maskb"], writes=[sbn])
                P.op("act", lambda e, sb_=sb_, i=i: e.activation(out=pT[:, i, :], in_=sb_[:], func=AF.Exp, scale=scale),
                     reads=[sbn], writes=["pT%d" % i])

            def omm(e):
                ins = None
                for i, (kt, mk) in enumerate(kbs):
                    ins = e.matmul(banks[4][:], lhsT=v_s[:, kt, g * 128:(g + 1) * 128], rhs=pT[:, i, :],
                                   start=(i == 0), stop=(i == len(kbs) - 1))
                return ins
            P.op("pe", omm, reads=["v"] + ["pT%d" % i for i in range(len(kbs))], writes=[bn[4]])

            def dmm(e):
                for i in range(len(kbs)):
                    e.matmul(banks[5][:], lhsT=onesb[:], rhs=pT[:, i, :], start=(i == 0), stop=False)
                return e.matmul(banks[5][:], lhsT=onesb[0:1, :], rhs=sinkb[0:1, g, :], start=False, stop=True)
            P.op("pe", dmm, reads=["onesb", "sinkb"] + ["pT%d" % i for i in range(len(kbs))], writes=[bn[5]])
            P.op("dve", lambda e: e.reciprocal(out=t2[:], in_=banks[5][:]), reads=[bn[5]], writes=["t2"])
            P.op("dve", lambda e, g=g: e.tensor_tensor(out=oT[:, g * 4:(g + 1) * 4, :],
                                                       in0=banks[4][:].rearrange("p (a b) -> p a b", a=4),
                                                       in1=t2[:].rearrange("p (a b) -> p a b", a=4), op=ALU.mult),
                 reads=[bn[4], "t2"], writes=["oT"])
        for dc in range(16):
            b = banks[6 + (dc % 2)]

            def pmm(e, b=b, dc=dc):
                ins = None
                for h in range(16):
                    ins = e.matmul(b[:, 0:128], lhsT=wo_s[:, h, dc * 128:(dc + 1) * 128], rhs=oT[:, h, :],
                                   start=(h == 0), stop=(h == 15))
                return ins
            P.op("pe", pmm, reads=["wo", "oT"], writes=[bn[6 + (dc % 2)]])
            P.op("dve", lambda e, b=b, dc=dc: e.scalar_tensor_tensor(out=sq[:, dc, :], in0=b[:, 0:128],
                                                                     scalar=G[:, dc, seg:seg + 1], in1=xt[:, dc, :],
                                                                     op0=ALU.mult, op1=ALU.add),
                 reads=[bn[6 + (dc % 2)], "G", "xt"], writes=["sq"])
        P.dma("sp", o_d[:, qi * 128:(qi + 1) * 128].rearrange("(k p) c -> p k c", p=128), sq[:], reads=["sq"])
    return P.finish()


def rope_tables(half):
    inv = 10000.0 ** (-np.arange(0, 64, 2, dtype=np.float64) / 64)
    tpos = np.arange(-128, 2048 + 128) + half * 2048
    rows, cols = tpos // 64, tpos % 64
    ang = np.concatenate([rows[:, None] * inv, rows[:, None] * inv, cols[:, None] * inv, cols[:, None] * inv], 1)
    cos = np.cos(ang).T.astype(np.float32)
    sin = np.sin(ang).T.astype(np.float32)

    def rep(tb):
        tb = tb.reshape(128, 18, 1, 128).transpose(1, 0, 2, 3)
        return np.ascontiguousarray(np.broadcast_to(tb, (18, 128, 4, 128)).reshape(18, 128, 512))
    return rep(cos), rep(sin)


def attn_consts(half):
    rm = np.zeros((128, 128), np.float32)
    for base in (0, 64):
        for p in range(32):
            rm[base + p + 32, base + p] = -1.0
            rm[base + p, base + p + 32] = 1.0
    j = np.arange(128)[:, None]
    i = np.arange(128)[None, :]
    NEG = -30000.0
    mp = np.where(j >= i, 0.0, NEG).astype(np.float32)
    mn = np.where(j <= i, 0.0, NEG).astype(np.float32)
    full = np.full((128, 128), NEG, np.float32)
    mp0 = full if half == 0 else mp
    mn15 = full if half == 1 else mn
    masks = np.stack([np.tile(m, (1, 4)) for m in (mp, mn, mp0, mn15)])
    cos, sin = rope_tables(half)
    return {"rmat": rm, "masks": np.ascontiguousarray(masks), "cosrep": cos, "sinrep": sin,
            "ident": np.eye(128, dtype=np.float32)}


def fm(v):
    return np.ascontiguousarray(np.asarray(v, np.float32).reshape(16, 128).T)


def fm2(v0, v1):
    return np.ascontiguousarray(np.stack([fm(v0), fm(v1)], -1))


def attn_inputs(xb, ctxb, half, nw, sh_l, sc_l, g_l, sh_c, sc_c, g_c, wqkv, wo, qg, kg, sinks):
    z = np.zeros((128, D), np.float32)
    prev = xb[half * 2048 - 128:half * 2048] if half == 1 else z
    nxt = xb[2048:2176] if half == 0 else z
    cols = np.concatenate([prev, xb[half * 2048:(half + 1) * 2048], nxt, ctxb], 0)
    d = {"xT": np.ascontiguousarray(cols.T), "nwT": fm(nw), "scT": fm2(sc_l, sc_c), "shT": fm2(sh_l, sh_c),
         "gT": fm2(g_l, g_c), "wqkv": wqkv, "wo": wo, "qg": np.ascontiguousarray(qg.reshape(128, 1)),
         "kg": np.ascontiguousarray(kg.reshape(128, 1)),
         "sinkrep": np.ascontiguousarray(np.repeat(np.asarray(sinks, np.float32), 128).reshape(1, 4, 512))}
    d.update(attn_consts(half))
    return d


I32 = mybir.dt.int32
S5_NTOK = 4352
TWO_PI = float(2 * np.pi)


def build_s5():
    P = Prog()
    NT = S5_NTOK // 128
    xT_d = P.dram_in("xT", [D, S5_NTOK])
    nw_d = P.dram_in("nwT", [128, 16])
    sc_d = P.dram_in("scT", [128, 16, 2])
    sh_d = P.dram_in("shT", [128, 16, 2])
    are_d = P.dram_in("are", [128, 64])
    aim_d = P.dram_in("aim", [128, 64])
    ldt_d = P.dram_in("ldt", [128, 64])
    bre_d = P.dram_in("bre", [64, 128, 128])
    bim_d = P.dram_in("bim", [64, 128, 128])
    cre_d = P.dram_in("cre", [64, 128, 128])
    cim_d = P.dram_in("cim", [64, 128, 128])
    iota_d = P.dram_in("iota", [128, 512])
    y_d = P.dram_out("yT", [D, S5_NTOK])

    rstd = P.sb("rstd", [128, S5_NTOK])
    uk = P.sb("uk", [128, S5_NTOK])
    xt = P.sb("xt", [128, 16, 128])
    sq = P.sb("sq", [128, 16, 128])
    ones32 = P.sb("ones32", [128, 128])
    iota = P.sb("iota", [128, 512])
    nw = P.sb("nwT", [128, 16])
    A = P.sb("A", [128, 16, 2])
    B = P.sb("B", [128, 16, 2])
    prm = {n: P.sb("p_" + n, [128, 64]) for n in
           ("are", "aim", "dt", "th", "r", "c1", "s1", "cr", "ci", "cT", "sT", "ta", "tb", "tc", "kf")}
    ki64 = P.sb("ki64", [128, 64], I32)
    tabs = [{n: P.sb("tab_%s%d" % (n, i), [128, 512]) for n in ("Wr", "Wi", "cos", "sin", "R")} for i in range(4)]
    wts = [{n: P.sb("w_%s%d" % (n, i), [128, 128]) for n in ("bre", "bim", "cre", "ncre", "ncim")} for i in range(4)]
    carry = [P.sb("carry%d" % i, [128, 4]) for i in range(4)]
    ki = P.sb("ki", [128, 512], I32)
    kf = P.sb("kf", [128, 512])
    WW = [dict([(n, P.sb("wk%d_%s" % (i, n), [128, 512])) for n in ("m1", "m2", "m3", "m4", "gr", "gi", "Gr", "Gi")] +
               [(n, P.sb("wk%d_%s" % (i, n), [128, 512], BF16)) for n in ("p1", "p2", "p3", "p4")]) for i in range(3)]
    W = WW[0]
    ukb = P.sb("ukb", [128, S5_NTOK], BF16)
    wtb = [{n: P.sb("wb_%s%d" % (n, i), [128, 128], BF16) for n in ("bre", "bim", "cre", "ncre", "ncim")} for i in range(4)]
    yo = [P.sb("yo%d" % i, [128, 512]) for i in range(2)]
    banks = [P.bank() for _ in range(8)]
    bn = ["bank%d" % i for i in range(8)]

    for dst, src, nm in ((nw, nw_d, "nw"), (A, sc_d, "A"), (B, sh_d, "B"), (iota, iota_d, "iota"),
                         (prm["are"], are_d, "are"), (prm["aim"], aim_d, "aim"), (prm["dt"], ldt_d, "dt")):
        P.dma("sp", dst[:], src, writes=[nm])
    P.op("dve", lambda e: e.memset(ones32[:], 1.0), writes=["ones32"])
    for s in range(2):
        P.op("dve", lambda e, s=s: e.scalar_tensor_tensor(out=A[:, :, s], in0=A[:, :, s], scalar=1.0, in1=nw[:],
                                                          op0=ALU.add, op1=ALU.mult), reads=["A", "nw"], writes=["A"])

    def ts(out, in0, s1, op0, s2=None, op1=None, rd=(), wr=()):
        if op1 is None:
            P.op("dve", lambda e: e.tensor_scalar(out=out, in0=in0, scalar1=s1, scalar2=None, op0=op0), reads=rd, writes=wr)
        else:
            P.op("dve", lambda e: e.tensor_scalar(out=out, in0=in0, scalar1=s1, scalar2=s2, op0=op0, op1=op1),
                 reads=rd, writes=wr)

    def tt(out, a, b, op, rd=(), wr=(), eng="dve"):
        P.op(eng, lambda e: e.tensor_tensor(out=out, in0=a, in1=b, op=op), reads=rd, writes=wr)

    def rr_sin(dst, dname, x, xname, kI, kF, kname, shift=0.0):
        if shift != 0.0:
            ts(x, x, shift, ALU.add, rd=[xname], wr=[xname])
        ts(kI, x, 1.0 / TWO_PI, ALU.mult, rd=[xname], wr=[kname + "i"])
        P.op("dve", lambda e: e.tensor_copy(out=kF, in_=kI), reads=[kname + "i"], writes=[kname + "f"])
        P.op("dve", lambda e: e.scalar_tensor_tensor(out=x, in0=kF, scalar=-TWO_PI, in1=x, op0=ALU.mult, op1=ALU.add),
             reads=[kname + "f", xname], writes=[xname])
        ts(x, x, 3.14159, ALU.min, -3.14159, ALU.max, rd=[xname], wr=[xname])
        P.op("act", lambda e: e.activation(out=dst, in_=x, func=AF.Sin), reads=[xname], writes=[dname])

    p = prm
    P.op("act", lambda e: e.activation(out=p["dt"][:], in_=p["dt"][:], func=AF.Exp), reads=["dt"], writes=["dt"])
    ts(p["are"][:], p["are"][:], -1e-4, ALU.min, rd=["are"], wr=["are"])
    tt(p["ta"][:], p["are"][:], p["dt"][:], ALU.mult, rd=["are", "dt"], wr=["ta"])
    P.op("act", lambda e: e.activation(out=p["r"][:], in_=p["ta"][:], func=AF.Exp), reads=["ta"], writes=["r"])
    tt(p["th"][:], p["aim"][:], p["dt"][:], ALU.mult, rd=["aim", "dt"], wr=["th"])
    ts(ki64[:], p["th"][:], 1.0 / TWO_PI, ALU.mult, rd=["th"], wr=["ki64"])
    P.op("dve", lambda e: e.tensor_copy(out=p["kf"][:], in_=ki64[:]), reads=["ki64"], writes=["kf64"])
    P.op("dve", lambda e: e.scalar_tensor_tensor(out=p["th"][:], in0=p["kf"][:], scalar=-TWO_PI, in1=p["th"][:],
                                                 op0=ALU.mult, op1=ALU.add), reads=["kf64", "th"], writes=["th"])
    P.op("dve", lambda e: e.tensor_copy(out=p["ta"][:], in_=p["th"][:]), reads=["th"], writes=["ta"])
    rr_sin(p["s1"][:], "s1", p["ta"][:], "ta", ki64[:], p["kf"][:], "k64")
    P.op("dve", lambda e: e.tensor_copy(out=p["ta"][:], in_=p["th"][:]), reads=["th", "s1"], writes=["ta"])
    rr_sin(p["c1"][:], "c1", p["ta"][:], "ta", ki64[:], p["kf"][:], "k64", shift=float(np.pi / 2))
    ts(p["ta"][:], p["th"][:], 512.0, ALU.mult, rd=["th", "c1"], wr=["ta"])
    rr_sin(p["sT"][:], "sT", p["ta"][:], "ta", ki64[:], p["kf"][:], "k64")
    ts(p["ta"][:], p["th"][:], 512.0, ALU.mult, rd=["th", "sT"], wr=["ta"])
    rr_sin(p["cT"][:], "cT", p["ta"][:], "ta", ki64[:], p["kf"][:], "k64", shift=float(np.pi / 2))
    tt(p["ta"][:], p["r"][:], p["c1"][:], ALU.mult, rd=["r", "c1", "cT"], wr=["ta"])
    ts(p["ta"][:], p["ta"][:], -1.0, ALU.add, rd=["ta"], wr=["ta"])
    tt(p["tb"][:], p["r"][:], p["s1"][:], ALU.mult, rd=["r", "s1"], wr=["tb"])
    tt(p["tc"][:], p["are"][:], p["are"][:], ALU.mult, rd=["are"], wr=["tc"])
    tt(p["kf"][:], p["aim"][:], p["aim"][:], ALU.mult, rd=["aim", "cT"], wr=["kf64"])
    tt(p["tc"][:], p["tc"][:], p["kf"][:], ALU.add, rd=["tc", "kf64"], wr=["tc"])
    P.op("dve", lambda e: e.reciprocal(out=p["tc"][:], in_=p["tc"][:]), reads=["tc"], writes=["tc"])
    tt(p["cr"][:], p["ta"][:], p["are"][:], ALU.mult, rd=["ta", "are"], wr=["cr"])
    tt(p["kf"][:], p["tb"][:], p["aim"][:], ALU.mult, rd=["tb", "aim"], wr=["kf64"])
    tt(p["cr"][:], p["cr"][:], p["kf"][:], ALU.add, rd=["cr", "kf64"], wr=["cr"])
    tt(p["cr"][:], p["cr"][:], p["tc"][:], ALU.mult, rd=["cr", "tc"], wr=["cr"])
    tt(p["ci"][:], p["tb"][:], p["are"][:], ALU.mult, rd=["tb", "are"], wr=["ci"])
    tt(p["kf"][:], p["ta"][:], p["aim"][:], ALU.mult, rd=["ta", "aim", "cr"], wr=["kf64"])
    tt(p["ci"][:], p["ci"][:], p["kf"][:], ALU.subtract, rd=["ci", "kf64"], wr=["ci"])
    tt(p["ci"][:], p["ci"][:], p["tc"][:], ALU.mult, rd=["ci", "tc"], wr=["ci"])
    PRM = ["th", "r", "cr", "ci", "cT", "sT"]

    for t in range(NT):
        c0 = t * 128
        P.dma("sp", xt[:], xT_d[:, c0:c0 + 128].rearrange("(k p) c -> p k c", p=128), writes=["xt"])
        P.op("act", lambda e: e.activation(out=sq[:].rearrange("p a b -> p (a b)"),
                                           in_=xt[:].rearrange("p a b -> p (a b)"), func=AF.Square),
             reads=["xt"], writes=["sq"])
        b = banks[t % 2]

        def mm(e, b=b):
            ins = None
            for k in range(16):
                ins = e.matmul(b[:, 0:128], lhsT=ones32[:], rhs=sq[:, k, :], start=(k == 0), stop=(k == 15))
            return ins
        P.op("pe", mm, reads=["sq", "ones32"], writes=[bn[t % 2]])
        P.op("act", lambda e, b=b, c0=c0: e.activation(out=rstd[:, c0:c0 + 128], in_=b[:, 0:128], func=AF.Sqrt,
                                                       scale=1.0 / D, bias=EPS), reads=[bn[t % 2]], writes=["rstd"])
    P.op("dve", lambda e: e.reciprocal(out=rstd[:], in_=rstd[:]), reads=["rstd"], writes=["rstd"])

    chunks = [(c, min(c + 512, S5_NTOK)) for c in range(0, S5_NTOK, 512)]
    yi = 0
    for k in range(16):
        P.dma("sp", uk[:], xT_d[k * 128:(k + 1) * 128, :], writes=["uk"])
        tt(uk[:], uk[:], rstd[:], ALU.mult, rd=["uk", "rstd"], wr=["uk"])
        for (a0, a1, seg) in ((0, 256, 1), (256, S5_NTOK, 0)):
            ts(ukb[:, a0:a1], uk[:, a0:a1], A[:, k, seg:seg + 1], ALU.mult, B[:, k, seg:seg + 1], ALU.add,
               rd=["uk", "A", "B"], wr=["ukb"])
        for j in range(4):
            q = 4 * k + j
            T_, Wt = tabs[j], wts[j]
            tn = "tab%d" % j
            for nm, src in (("bre", bre_d), ("bim", bim_d), ("cre", cre_d), ("ncim", cim_d)):
                P.dma("sp", Wt[nm][:], src[q], writes=["w%d" % j])
            Wb = wtb[j]
            ts(Wb["bre"][:], Wt["bre"][:], 1.0, ALU.mult, rd=["w%d" % j], wr=["wb%d" % j])
            ts(Wb["bim"][:], Wt["bim"][:], 1.0, ALU.mult, rd=["w%d" % j], wr=["wb%d" % j])
            ts(Wb["cre"][:], Wt["cre"][:], 1.0, ALU.mult, rd=["w%d" % j], wr=["wb%d" % j])
            ts(Wb["ncre"][:], Wt["cre"][:], -1.0, ALU.mult, rd=["w%d" % j], wr=["wb%d" % j])
            ts(Wb["ncim"][:], Wt["ncim"][:], -1.0, ALU.mult, rd=["w%d" % j], wr=["wb%d" % j])
            thq = p["th"][:, q:q + 1]
            ts(W["m1"][:], iota[:], thq, ALU.mult, rd=["iota"] + PRM, wr=["m1"])
            rr_sin(T_["sin"][:], tn, W["m1"][:], "m1", ki[:], kf[:], "kk")
            ts(W["m1"][:], iota[:], thq, ALU.mult, rd=["iota", tn], wr=["m1"])
            rr_sin(T_["cos"][:], tn, W["m1"][:], "m1", ki[:], kf[:], "kk", shift=float(np.pi / 2))
            ts(W["m2"][:], T_["sin"][:], p["ci"][:, q:q + 1], ALU.mult, rd=[tn], wr=["m2"])
            P.op("dve", lambda e, T_=T_, q=q: e.scalar_tensor_tensor(out=T_["Wr"][:], in0=T_["cos"][:],
                                                                     scalar=p["cr"][:, q:q + 1], in1=W["m2"][:],
                                                                     op0=ALU.mult, op1=ALU.add),
                 reads=[tn, "m2"], writes=[tn])
            ts(W["m2"][:], T_["sin"][:], p["cr"][:, q:q + 1], ALU.mult, rd=[tn], wr=["m2"])
            P.op("dve", lambda e, T_=T_, q=q: e.scalar_tensor_tensor(out=T_["Wi"][:], in0=T_["cos"][:],
                                                                     scalar=p["ci"][:, q:q + 1], in1=W["m2"][:],
                                                                     op0=ALU.mult, op1=ALU.subtract),
                 reads=[tn, "m2"], writes=[tn])
            ts(T_["R"][:], iota[:], 0.0, ALU.mult, p["r"][:, q:q + 1], ALU.add, rd=["iota", tn], wr=[tn])
            P.op("dve", lambda e, j=j: e.memset(carry[j][:], 0.0), writes=["carry%d" % j])
        units = [(ci, j) for ci in range(len(chunks)) for j in range(4)]
        U = len(units)

        def S1(u):
            ci, j = units[u]
            c0, c1 = chunks[ci]
            n = c1 - c0
            sx = u % 3
            W = WW[sx]
            T_, Wb = tabs[j], wtb[j]
            tn, wn = "tab%d" % j, "wb%d" % j
            bR, bI = 2 + 2 * sx, 3 + 2 * sx
            P.op("pe", lambda e: e.matmul(banks[bR][:, 0:n], lhsT=Wb["bre"][:], rhs=ukb[:, c0:c1], start=True, stop=True),
                 reads=[wn, "ukb"], writes=[bn[bR]])
            P.op("pe", lambda e: e.matmul(banks[bI][:, 0:n], lhsT=Wb["bim"][:], rhs=ukb[:, c0:c1], start=True, stop=True),
                 reads=[wn, "ukb"], writes=[bn[bI]])
            Pr, Pi = banks[bR][:, 0:n], banks[bI][:, 0:n]
            tt(W["m1"][:, 0:n], Pr, T_["Wr"][:, 0:n], ALU.mult, rd=[bn[bR], tn], wr=["m1_%d" % sx])
            tt(W["m2"][:, 0:n], Pi, T_["Wi"][:, 0:n], ALU.mult, rd=[bn[bI], tn], wr=["m2_%d" % sx])
            tt(W["m3"][:, 0:n], Pi, T_["Wr"][:, 0:n], ALU.mult, rd=[bn[bI], tn], wr=["m3_%d" % sx])
            tt(W["m4"][:, 0:n], Pr, T_["Wi"][:, 0:n], ALU.mult, rd=[bn[bR], tn], wr=["m4_%d" % sx])
            tt(W["gr"][:, 0:n], W["m1"][:, 0:n], W["m2"][:, 0:n], ALU.subtract, rd=["m1_%d" % sx, "m2_%d" % sx],
               wr=["gr_%d" % sx], eng="pool")
            tt(W["gi"][:, 0:n], W["m3"][:, 0:n], W["m4"][:, 0:n], ALU.add, rd=["m3_%d" % sx, "m4_%d" % sx],
               wr=["gi_%d" % sx], eng="pool")

        def S2(u):
            ci, j = units[u]
            c0, c1 = chunks[ci]
            n = c1 - c0
            sx = u % 3
            W = WW[sx]
            q = 4 * k + j
            T_ = tabs[j]
            tn, cn = "tab%d" % j, "carry%d" % j
            P.op("dve", lambda e: e.tensor_tensor_scan(out=W["Gr"][:, 0:n], data0=T_["R"][:, 0:n], data1=W["gr"][:, 0:n],
                                                       initial=carry[j][:, 0:1], op0=ALU.mult, op1=ALU.add),
                 reads=[tn, "gr_%d" % sx, cn], writes=["Gr_%d" % sx])
            P.op("dve", lambda e: e.tensor_tensor_scan(out=W["Gi"][:, 0:n], data0=T_["R"][:, 0:n], data1=W["gi"][:, 0:n],
                                                       initial=carry[j][:, 1:2], op0=ALU.mult, op1=ALU.add),
                 reads=[tn, "gi_%d" % sx, cn], writes=["Gi_%d" % sx])
            if n == 512:
                gl_r, gl_i = W["Gr"][:, 511:512], W["Gi"][:, 511:512]
                ts(carry[j][:, 2:3], gl_i, p["sT"][:, q:q + 1], ALU.mult, rd=["Gi_%d" % sx] + PRM, wr=[cn])
                P.op("dve", lambda e: e.scalar_tensor_tensor(out=carry[j][:, 0:1], in0=gl_r, scalar=p["cT"][:, q:q + 1],
                                                             in1=carry[j][:, 2:3], op0=ALU.mult, op1=ALU.subtract),
                     reads=["Gr_%d" % sx, cn], writes=[cn])
                ts(carry[j][:, 3:4], gl_i, p["cT"][:, q:q + 1], ALU.mult, rd=["Gi_%d" % sx], wr=[cn])
                P.op("dve", lambda e: e.scalar_tensor_tensor(out=carry[j][:, 1:2], in0=gl_r, scalar=p["sT"][:, q:q + 1],
                                                             in1=carry[j][:, 3:4], op0=ALU.mult, op1=ALU.add),
                     reads=["Gr_%d" % sx, cn], writes=[cn])
            tt(W["p1"][:, 0:n], W["Gr"][:, 0:n], T_["cos"][:, 0:n], ALU.mult, rd=["Gr_%d" % sx, tn], wr=["p1_%d" % sx])
            tt(W["p2"][:, 0:n], W["Gi"][:, 0:n], T_["sin"][:, 0:n], ALU.mult, rd=["Gi_%d" % sx, tn], wr=["p2_%d" % sx])
            tt(W["p3"][:, 0:n], W["Gr"][:, 0:n], T_["sin"][:, 0:n], ALU.mult, rd=["Gr_%d" % sx, tn], wr=["p3_%d" % sx],
               eng="pool")
            tt(W["p4"][:, 0:n], W["Gi"][:, 0:n], T_["cos"][:, 0:n], ALU.mult, rd=["Gi_%d" % sx, tn], wr=["p4_%d" % sx],
               eng="pool")

        def S3(u):
            nonlocal yi
            ci, j = units[u]
            c0, c1 = chunks[ci]
            n = c1 - c0
            sx = u % 3
            W = WW[sx]
            Wb = wtb[j]
            wn = "wb%d" % j
            yb = banks[yi % 2]
            ybn = bn[yi % 2]

            def cmm(e):
                e.matmul(yb[:, 0:n], lhsT=Wb["cre"][:], rhs=W["p1"][:, 0:n], start=(j == 0), stop=False)
                e.matmul(yb[:, 0:n], lhsT=Wb["ncre"][:], rhs=W["p2"][:, 0:n], start=False, stop=False)
                e.matmul(yb[:, 0:n], lhsT=Wb["ncim"][:], rhs=W["p3"][:, 0:n], start=False, stop=False)
                return e.matmul(yb[:, 0:n], lhsT=Wb["ncim"][:], rhs=W["p4"][:, 0:n], start=False, stop=(j == 3))
            P.op("pe", cmm, reads=[wn] + ["p%d_%d" % (i, sx) for i in (1, 2, 3, 4)], writes=[ybn])
            if j == 3:
                yo_ = yo[yi % 2]
                P.op("act", lambda e: e.activation(out=yo_[:, 0:n], in_=yb[:, 0:n], func=AF.Identity),
                     reads=[ybn], writes=["yo%d" % (yi % 2)])
                P.dma("sp", y_d[k * 128:(k + 1) * 128, c0:c1], yo_[:, 0:n], reads=["yo%d" % (yi % 2)])
                yi += 1
        for step in range(U + 2):
            if step < U:
                S1(step)
            if 0 <= step - 1 < U:
                S2(step - 1)
            if 0 <= step - 2 < U:
                S3(step - 2)
    return P.finish()


def s5_inputs(xT_scan, nw, sh_l, sc_l, sh_c, sc_c, a_re, a_im, log_dt, b_re, b_im, c_re, c_im):
    def pl(a):
        return np.ascontiguousarray(a.reshape(64, 2, 64).transpose(1, 2, 0).reshape(128, 64))
    ldt = np.broadcast_to(log_dt[:, None], (128, 64))
    Bre = np.zeros((64, 128, 128), np.float32)
    Bim = np.zeros_like(Bre)
    Cre = np.zeros_like(Bre)
    Cim = np.zeros_like(Bre)
    for q in range(64):
        for gi in range(2):
            g = 2 * q + gi
            r0 = (q % 4) * 32 + gi * 16
            Bre[q, r0:r0 + 16, gi * 64:(gi + 1) * 64] = b_re[g].T
            Bim[q, r0:r0 + 16, gi * 64:(gi + 1) * 64] = b_im[g].T
            Cre[q, gi * 64:(gi + 1) * 64, r0:r0 + 16] = c_re[g].T
            Cim[q, gi * 64:(gi + 1) * 64, r0:r0 + 16] = c_im[g].T
    return {"xT": xT_scan, "nwT": fm(nw), "scT": fm2(sc_l, sc_c), "shT": fm2(sh_l, sh_c),
            "are": pl(a_re), "aim": pl(a_im), "ldt": pl(ldt), "bre": Bre, "bim": Bim, "cre": Cre, "cim": Cim,
            "iota": np.ascontiguousarray(np.broadcast_to(np.arange(512, dtype=np.float32)[None, :], (128, 512)))}


def build_glu():
    P = Prog()
    NTOK = 18 * 128
    xT_d = P.dram_in("xT", [D, NTOK])
    yf_d = P.dram_in("yfT", [D, NTOK])
    yb_d = P.dram_in("ybT", [D, NTOK])
    nw_d = P.dram_in("nwT", [128, 16])
    sc_d = P.dram_in("scT", [128, 16, 2])
    sh_d = P.dram_in("shT", [128, 16, 2])
    g_d = P.dram_in("gT", [128, 16, 2])
    dsk_d = P.dram_in("dskT", [128, 16])
    wg_d = P.dram_in("wglu", [D, 2 * D])
    bg_d = P.dram_in("bgluT", [128, 32])
    o_d = P.dram_out("xoT", [D, NTOK])

    wg = P.sb("wg", [128, 16, 2 * D], BF16)
    xt = P.sb("xt", [128, 16, 128])
    sq = P.sb("sq", [128, 16, 128])
    yf = P.sb("yf", [128, 16, 128])
    yb = P.sb("yb", [128, 16, 128])
    gT = P.sb("gT_", [128, 16, 128], BF16)
    sg = [P.sb("sg%d" % i, [128, 128]) for i in range(2)]
    ot = [P.sb("ot%d" % i, [128, 128]) for i in range(2)]
    rsd = P.sb("rsd", [128, 128])
    ones32 = P.sb("ones32", [128, 128])
    nw = P.sb("nwT", [128, 16])
    A = P.sb("A", [128, 16, 2])
    B = P.sb("B", [128, 16, 2])
    G = P.sb("G", [128, 16, 2])
    dsk = P.sb("dsk", [128, 16])
    bg = P.sb("bg", [128, 32])
    banks = [P.bank() for _ in range(8)]
    bn = ["bank%d" % i for i in range(8)]
    for dst, src, nm in ((nw, nw_d, "nw"), (A, sc_d, "A"), (B, sh_d, "B"), (G, g_d, "G"), (dsk, dsk_d, "dsk"),
                         (bg, bg_d, "bg")):
        P.dma("sp", dst[:], src, writes=[nm])
    P.op("dve", lambda e: e.memset(ones32[:], 1.0), writes=["ones32"])
    for s in range(2):
        P.op("dve", lambda e, s=s: e.scalar_tensor_tensor(out=A[:, :, s], in0=A[:, :, s], scalar=1.0, in1=nw[:],
                                                          op0=ALU.add, op1=ALU.mult), reads=["A", "nw"], writes=["A"])
    for cb in range(8):
        P.dma("pool", wg[:, :, cb * 512:(cb + 1) * 512],
              wg_d[:, cb * 512:(cb + 1) * 512].rearrange("(k p) c -> p k c", p=128), writes=["wg"])

    def flat(t):
        return t[:].rearrange("p a b -> p (a b)")
    for t in range(18):
        seg = 0 if t < 16 else 1
        c0 = t * 128
        P.dma("sp", xt[:], xT_d[:, c0:c0 + 128].rearrange("(k p) c -> p k c", p=128), writes=["xt"])
        P.dma("sp", yf[:], yf_d[:, c0:c0 + 128].rearrange("(k p) c -> p k c", p=128), writes=["yf"])
        P.dma("sp", yb[:], yb_d[:, c0:c0 + 128].rearrange("(k p) c -> p k c", p=128), writes=["yb"])
        P.op("act", lambda e: e.activation(out=flat(sq), in_=flat(xt), func=AF.Square), reads=["xt"], writes=["sq"])

        def mm(e):
            ins = None
            for k in range(16):
                ins = e.matmul(banks[7][:, 0:128], lhsT=ones32[:], rhs=sq[:, k, :], start=(k == 0), stop=(k == 15))
            return ins
        P.op("pe", mm, reads=["sq", "ones32"], writes=[bn[7]])
        P.op("act", lambda e: e.activation(out=rsd[:], in_=banks[7][:, 0:128], func=AF.Sqrt, scale=1.0 / D, bias=EPS),
             reads=[bn[7]], writes=["rsd"])
        P.op("dve", lambda e: e.reciprocal(out=rsd[:], in_=rsd[:]), reads=["rsd"], writes=["rsd"])
        for k in range(16):
            P.op("dve", lambda e, k=k: e.scalar_tensor_tensor(out=sq[:, k, :], in0=xt[:, k, :],
                                                              scalar=A[:, k, seg:seg + 1], in1=rsd[:],
                                                              op0=ALU.mult, op1=ALU.mult),
                 reads=["xt", "A", "rsd"], writes=["sq"])
            P.op("dve", lambda e, k=k: e.tensor_scalar(out=sq[:, k, :], in0=sq[:, k, :], scalar1=B[:, k, seg:seg + 1],
                                                       scalar2=dsk[:, k:k + 1], op0=ALU.add, op1=ALU.mult),
                 reads=["sq", "B", "dsk"], writes=["sq"])
        P.op("dve", lambda e: e.tensor_tensor(out=flat(sq), in0=flat(sq), in1=flat(yf), op=ALU.add),
             reads=["sq", "yf"], writes=["sq"])
        P.op("dve", lambda e: e.tensor_tensor(out=flat(sq), in0=flat(sq), in1=flat(yb), op=ALU.add),
             reads=["sq", "yb"], writes=["sq"])
        P.op("act", lambda e: e.activation(out=flat(yf), in_=flat(sq), func=AF.Square), reads=["sq"], writes=["yf"])
        P.op("dve", lambda e: e.tensor_scalar(out=flat(yf), in0=flat(yf), scalar1=0.044715, scalar2=1.0,
                                              op0=ALU.mult, op1=ALU.add), reads=["yf"], writes=["yf"])
        P.op("dve", lambda e: e.tensor_tensor(out=flat(yf), in0=flat(yf), in1=flat(sq), op=ALU.mult),
             reads=["yf", "sq"], writes=["yf"])
        P.op("act", lambda e: e.activation(out=flat(yf), in_=flat(yf), func=AF.Sigmoid, scale=1.5957691216057308),
             reads=["yf"], writes=["yf"])
        P.op("dve", lambda e: e.tensor_tensor(out=flat(gT), in0=flat(yf), in1=flat(sq), op=ALU.mult),
             reads=["yf", "sq"], writes=["gT"])
        for dc in range(16):
            i2 = dc % 2
            b1, b2 = banks[2 * i2], banks[2 * i2 + 1]

            def zmm(e, b, c):
                ins = None
                for k in range(16):
                    ins = e.matmul(b[:, 0:128], lhsT=wg[:, k, c:c + 128], rhs=gT[:, k, :], start=(k == 0), stop=(k == 15))
                return ins
            P.op("pe", lambda e: zmm(e, b1, dc * 128), reads=["wg", "gT"], writes=[bn[2 * i2]])
            P.op("pe", lambda e: zmm(e, b2, D + dc * 128), reads=["wg", "gT"], writes=[bn[2 * i2 + 1]])
            P.op("act", lambda e: e.activation(out=sg[i2][:], in_=b2[:, 0:128], func=AF.Sigmoid,
                                               bias=bg[:, 16 + dc:17 + dc]),
                 reads=[bn[2 * i2 + 1], "bg"], writes=["sg%d" % i2])
            P.op("dve", lambda e: e.scalar_tensor_tensor(out=ot[i2][:], in0=b1[:, 0:128], scalar=bg[:, dc:dc + 1],
                                                         in1=sg[i2][:], op0=ALU.add, op1=ALU.mult),
                 reads=[bn[2 * i2], "bg", "sg%d" % i2], writes=["ot%d" % i2])
            P.op("dve", lambda e: e.scalar_tensor_tensor(out=yb[:, dc, :], in0=ot[i2][:], scalar=G[:, dc, seg:seg + 1],
                                                         in1=xt[:, dc, :], op0=ALU.mult, op1=ALU.add),
                 reads=["ot%d" % i2, "G", "xt"], writes=["yb"])
        P.dma("sp", o_d[:, c0:c0 + 128].rearrange("(k p) c -> p k c", p=128), yb[:], reads=["yb"])
    return P.finish()


_PROGS = {}


def _prog(name, fn):
    if name not in _PROGS:
        _PROGS[name] = fn()
    return _PROGS[name]


def _run(nc, maps):
    return run_bass_kernel_spmd(nc, maps, core_ids=list(range(8))).results


def kernel(x, c, ctx, c_ctx, w_mod, b_mod, norm_mix, norm_ffn, w_router, b_router, w_gate_up, b_gate_up,
           w_down, b_down, attn_w_qkv, attn_w_o, attn_q_gain, attn_k_gain, attn_sinks, ssm_a_re, ssm_a_im,
           ssm_log_dt, ssm_b_re, ssm_b_im, ssm_c_re, ssm_c_im, ssm_d, ssm_w_glu, ssm_b_glu):
    f = lambda a: np.asarray(a, np.float32)
    x = f(x).copy()
    ctx = f(ctx).copy()
    mod = run_mod(f(c), f(c_ctx), f(w_mod), f(b_mod)).reshape(4, 5, 6, D)
    SH_M, SC_M, G_M, SH_F, SC_F, G_F = range(6)
    for i in range(4):
        j = i // 2
        m = mod[i]
        if i % 2 == 0:
            nc = _prog("attn", build_attn)
            maps = []
            for core in range(8):
                b, hf = core // 2, core % 2
                maps.append(attn_inputs(x[b], ctx[b], hf, f(norm_mix[i]), m[b, SH_M], m[b, SC_M], m[b, G_M],
                                        m[4, SH_M], m[4, SC_M], m[4, G_M], f(attn_w_qkv[j]), f(attn_w_o[j]),
                                        f(attn_q_gain[j]), f(attn_k_gain[j]), f(attn_sinks[j])))
            res = _run(nc, maps)
            for core in range(8):
                b, hf = core // 2, core % 2
                xo = res[core]["xoT"]
                x[b, hf * 2048:(hf + 1) * 2048] = xo[:, :2048].T
                if hf == 0:
                    ctx[b] = xo[:, 2048:].T
        else:
            nc = _prog("s5", build_s5)
            maps = []
            for core in range(8):
                b, d = core // 2, core % 2
                seq = np.concatenate([ctx[b], x[b]], 0) if d == 0 else np.concatenate([ctx[b][::-1], x[b][::-1]], 0)
                maps.append(s5_inputs(np.ascontiguousarray(seq.T), f(norm_mix[i]), m[b, SH_M], m[b, SC_M],
                                      m[4, SH_M], m[4, SC_M], f(ssm_a_re[j, d]), f(ssm_a_im[j, d]),
                                      f(ssm_log_dt[j, d]), f(ssm_b_re[j, d]), f(ssm_b_im[j, d]),
                                      f(ssm_c_re[j, d]), f(ssm_c_im[j, d])))
            res = _run(nc, maps)
            ydir = {}
            for core in range(8):
                b, d = core // 2, core % 2
                y = res[core]["yT"].T
                yc, yl = y[:256], y[256:]
                if d == 1:
                    yc, yl = yc[::-1], yl[::-1]
                ydir[(b, d)] = (yc, yl)
            nc = _prog("glu", build_glu)
            maps = []
            for core in range(8):
                b, hf = core // 2, core % 2
                sl = slice(hf * 2048, (hf + 1) * 2048)
                cat = lambda lat, cx: np.ascontiguousarray(np.concatenate([lat, cx], 0).T)
                maps.append({
                    "xT": cat(x[b, sl], ctx[b]),
                    "yfT": cat(ydir[(b, 0)][1][sl], ydir[(b, 0)][0]),
                    "ybT": cat(ydir[(b, 1)][1][sl], ydir[(b, 1)][0]),
                    "nwT": fm(norm_mix[i]), "scT": fm2(m[b, SC_M], m[4, SC_M]), "shT": fm2(m[b, SH_M], m[4, SH_M]),
                    "gT": fm2(m[b, G_M], m[4, G_M]), "dskT": fm(ssm_d[j]), "wglu": f(ssm_w_glu[j]),
                    "bgluT": np.ascontiguousarray(f(ssm_b_glu[j]).reshape(32, 128).T),
                })
            res = _run(nc, maps)
            for core in range(8):
                b, hf = core // 2, core % 2
                xo = res[core]["xoT"]
                x[b, hf * 2048:(hf + 1) * 2048] = xo[:, :2048].T
                if hf == 0:
                    ctx[b] = xo[:, 2048:].T
        last = i == 3
        if last:
            nc = _prog("moe16", lambda: build_moe(NT=16, n_lat=16, passes=[list(range(p * 4, p * 4 + 4)) for p in range(4)]))
        else:
            nc = _prog("moe", build_moe)
        maps = []
        for core in range(8):
            b, hf = core // 2, core % 2
            xt = x[b, hf * 2048:(hf + 1) * 2048]
            if not last:
                xt = np.concatenate([xt, ctx[b, hf * 128:(hf + 1) * 128]], 0)
            maps.append(moe_inputs(xt, f(norm_ffn[i]), np.stack([m[b, SC_F], m[4, SC_F]]),
                                   np.stack([m[b, SH_F], m[4, SH_F]]), np.stack([m[b, G_F], m[4, G_F]]),
                                   f(w_router[i]), f(b_router[i]), f(w_gate_up[i]), f(b_gate_up[i]),
                                   f(w_down[i]), f(b_down[i])))
        res = _run(nc, maps)
        for core in range(8):
            b, hf = core // 2, core % 2
            y = res[core]["y"]
            x[b, hf * 2048:(hf + 1) * 2048] = y[:2048]
            if not last:
                ctx[b, hf * 128:(hf + 1) * 128] = y[2048:]
    return x
```
